# Optimizing a Trainium2 kernel written in Bass

```python
import jax, jax.numpy as jnp
from jax import lax
import numpy as np

D_MODEL = 1024
BATCH = 2
SEQ = 16384
DEPTH = 2

N_A = (DEPTH + 1) // 2
N_B = DEPTH // 2

HEAD_DIM = 64
A_GROUPS = 3
A_HEADS_PER_GROUP = 5
A_HEADS = A_GROUPS * A_HEADS_PER_GROUP
A_WIDTH = A_HEADS * HEAD_DIM
DILATED_PATTERNS = ((128, 1), (512, 4), (2048, 16))
QUERY_BLOCK = 128
ROT_DIM = HEAD_DIM // 4
ROPE_THETA = 500000.0
NEG_INF = -1e30

CHUNK = 128
GMLP_HALF = 2 * D_MODEL
GMLP_GROUPS = 8

N_EXPERT_GROUPS = 4
EXPERTS_PER_GROUP = 4
N_EXPERTS = N_EXPERT_GROUPS * EXPERTS_PER_GROUP
TOP_K_INNER = 2
D_EXPERT = 256

EPS = 1e-6

kernel_name = "hybrid_dilated_attn_gmlp_hmoe_encoder"


def rms_norm(x, g):
    xf = x.astype(jnp.float32)
    y = xf * lax.rsqrt(jnp.mean(xf * xf, axis=-1, keepdims=True) + EPS)
    return (y * g.astype(jnp.float32)).astype(x.dtype)


def layer_norm(x, g):
    xf = x.astype(jnp.float32)
    mu = jnp.mean(xf, axis=-1, keepdims=True)
    xc = xf - mu
    y = xc * lax.rsqrt(jnp.mean(xc * xc, axis=-1, keepdims=True) + EPS)
    return (y * g.astype(jnp.float32)).astype(x.dtype)


def modulate(h, shift, scale):
    return (h * (1.0 + scale[:, None, :]) + shift[:, None, :]).astype(h.dtype)


def partial_rope(t, pos):
    inv_freq = ROPE_THETA ** (-jnp.arange(0, ROT_DIM, 2, dtype=jnp.float32) / ROT_DIM)
    ang = pos.astype(jnp.float32)[:, None] * inv_freq[None, :]
    cos = jnp.concatenate([jnp.cos(ang), jnp.cos(ang)], -1)[None, :, None, :]
    sin = jnp.concatenate([jnp.sin(ang), jnp.sin(ang)], -1)[None, :, None, :]
    rot, rest = t[..., :ROT_DIM], t[..., ROT_DIM:]
    r1, r2 = rot[..., :ROT_DIM // 2], rot[..., ROT_DIM // 2:]
    rot_half = jnp.concatenate([-r2, r1], -1)
    rot = rot * cos + rot_half * sin
    return jnp.concatenate([rot.astype(t.dtype), rest], -1)


def banded_dilated_attention(q, k, v, dilation, radius):
    B, S, H, Dh = q.shape
    L = S // dilation
    Lp = -(-L // QUERY_BLOCK) * QUERY_BLOCK
    n_blk = Lp // QUERY_BLOCK

    def to_sub(t):
        return t.reshape(B, L, dilation, H, Dh).transpose(0, 2, 1, 3, 4)

    qs = jnp.pad(to_sub(q), ((0, 0), (0, 0), (0, Lp - L), (0, 0), (0, 0)))
    qs = qs.reshape(B, dilation, n_blk, QUERY_BLOCK, H, Dh)

    def key_blocks(t):
        tp = jnp.pad(to_sub(t), ((0, 0), (0, 0), (radius, Lp + QUERY_BLOCK - L - radius), (0, 0), (0, 0)))
        tp = tp.reshape(B, dilation, n_blk + 1, QUERY_BLOCK, H, Dh)
        return jnp.concatenate([tp[:, :, :-1], tp[:, :, 1:]], axis=3)

    kb, vb = key_blocks(k), key_blocks(v)
    s = jnp.einsum('bdnqhc,bdnkhc->bdnqhk', qs, kb).astype(jnp.float32) * (Dh ** -0.5)

    t_idx = jnp.arange(QUERY_BLOCK)[:, None]
    k_idx = jnp.arange(2 * QUERY_BLOCK)[None, :]
    rel = k_idx - t_idx
    band = (rel >= 0) & (rel <= 2 * radius)
    key_pos = jnp.arange(n_blk)[:, None] * QUERY_BLOCK + jnp.arange(2 * QUERY_BLOCK)[None, :] - radius
    kvalid = (key_pos >= 0) & (key_pos < L)
    mask = band[None, :, :] & kvalid[:, None, :]
    s = jnp.where(mask[None, None, :, :, None, :], s, NEG_INF)

    m = jnp.max(s, axis=-1, keepdims=True)
    p = jnp.exp(s - m)
    denom = jnp.sum(p, axis=-1)
    o = jnp.einsum('bdnqhk,bdnkhc->bdnqhc', p, vb.astype(jnp.float32)) / denom[..., None]
    lse = m[..., 0] + jnp.log(denom)

    o = o.reshape(B, dilation, Lp, H, Dh)[:, :, :L].transpose(0, 2, 1, 3, 4).reshape(B, S, H, Dh)
    lse = lse.reshape(B, dilation, Lp, H)[:, :, :L].transpose(0, 2, 1, 3).reshape(B, S, H)
    return o, lse


def mixer_dilated_attention(h, w_qkv, w_o):
    B, S, _ = h.shape
    qkv = (h @ w_qkv).reshape(B, S, 3, A_HEADS, HEAD_DIM)
    pos = jnp.arange(S)
    q = partial_rope(qkv[:, :, 0], pos)
    k = partial_rope(qkv[:, :, 1], pos)
    v = qkv[:, :, 2]
    outs, lses = [], []
    for g, (window, dil) in enumerate(DILATED_PATTERNS):
        sl = slice(g * A_HEADS_PER_GROUP, (g + 1) * A_HEADS_PER_GROUP)
        o, l = banded_dilated_attention(q[:, :, sl], k[:, :, sl], v[:, :, sl], dil, window // (2 * dil))
        outs.append(o)
        lses.append(l)
    o = jnp.stack(outs, axis=2)
    lse = jnp.stack(lses, axis=2)
    alpha = jax.nn.softmax(lse, axis=2)
    o = (o * alpha[..., None]).astype(h.dtype).reshape(B, S, A_WIDTH)
    return o @ w_o


def mixer_spatial_gating(h, w_in, v_gain, w_s, b_s, w_o):
    B, S, _ = h.shape
    z = jax.nn.gelu(h @ w_in, approximate=False)
    u, v = z[..., :GMLP_HALF], z[..., GMLP_HALF:]
    v = layer_norm(v, v_gain)
    vc = v.reshape(B, S // CHUNK, CHUNK, GMLP_GROUPS, GMLP_HALF // GMLP_GROUPS)
    vs = jnp.einsum('gpq,bnqgc->bnpgc', w_s, vc) + jnp.transpose(b_s)[None, None, :, :, None]
    return (u * vs.reshape(B, S, GMLP_HALF).astype(u.dtype)) @ w_o


def hierarchical_moe(h, w_group, b_group, w_expert, b_expert, w_gate, w_up, w_down):
    B, S, D = h.shape
    T = B * S
    ht = h.reshape(T, D)
    g_logits = (ht @ w_group + b_group).astype(jnp.float32)
    g_prob = jax.nn.softmax(g_logits, axis=-1)
    g_val, g_idx = lax.top_k(g_prob, 1)
    e_all = (jnp.einsum('td,gde->tge', ht, w_expert) + b_expert).astype(jnp.float32)
    e_logits = jnp.take_along_axis(e_all, g_idx[:, :, None], axis=1)[:, 0]
    top_v, top_i = lax.top_k(e_logits, TOP_K_INNER)
    top_p = jax.nn.softmax(top_v, axis=-1)
    w_inner = jnp.sum(jax.nn.one_hot(top_i, EXPERTS_PER_GROUP, dtype=jnp.float32) * top_p[..., None], axis=1)
    gate = jax.nn.one_hot(g_idx[:, 0], N_EXPERT_GROUPS, dtype=jnp.float32)[:, :, None] * (g_val[:, :, None] * w_inner[:, None, :])
    gate = gate.reshape(T, N_EXPERTS)
    hg = jnp.einsum('td,edf->tef', ht, w_gate)
    hu = jnp.einsum('td,edf->tef', ht, w_up)
    act = (jax.nn.silu(hg) * hu * gate[..., None]).astype(h.dtype)
    y = jnp.einsum('tef,efd->td', act, w_down)
    return y.reshape(B, S, D)


def setup_inputs(seed: int = 0) -> dict:
    key = jax.random.key(seed)
    ks = jax.random.split(key, 24)
    f32 = jnp.float32

    def nrm(k, shape, fan_in, s=1.0):
        return jax.random.normal(k, shape, f32) * (s * fan_in ** -0.5)

    def gain(k, shape):
        return 1.0 + 0.02 * jax.random.normal(k, shape, f32)

    D = D_MODEL
    return {
        "x": jax.random.normal(ks[0], (BATCH, SEQ, D), f32),
        "c": jax.random.normal(ks[1], (BATCH, D), f32),
        "norm_mix": gain(ks[2], (DEPTH, D)),
        "norm_ffn": gain(ks[3], (DEPTH, D)),
        "w_ada": nrm(ks[4], (DEPTH, D, 6 * D), D, 0.5),
        "b_ada": 0.01 * jax.random.normal(ks[5], (DEPTH, 6 * D), f32),
        "a_w_qkv": nrm(ks[6], (N_A, D, 3 * A_WIDTH), D),
        "a_w_o": nrm(ks[7], (N_A, A_WIDTH, D), A_WIDTH),
        "b_w_in": nrm(ks[8], (N_B, D, 2 * GMLP_HALF), D),
        "b_v_gain": gain(ks[9], (N_B, GMLP_HALF)),
        "b_w_s": nrm(ks[10], (N_B, GMLP_GROUPS, CHUNK, CHUNK), CHUNK),
        "b_b_s": 1.0 + 0.01 * jax.random.normal(ks[11], (N_B, GMLP_GROUPS, CHUNK), f32),
        "b_w_o": nrm(ks[12], (N_B, GMLP_HALF, D), GMLP_HALF),
        "r_w_group": nrm(ks[13], (DEPTH, D, N_EXPERT_GROUPS), D),
        "r_b_group": 0.01 * jax.random.normal(ks[14], (DEPTH, N_EXPERT_GROUPS), f32),
        "r_w_expert": nrm(ks[15], (DEPTH, N_EXPERT_GROUPS, D, EXPERTS_PER_GROUP), D),
        "r_b_expert": 0.01 * jax.random.normal(ks[16], (DEPTH, N_EXPERT_GROUPS, EXPERTS_PER_GROUP), f32),
        "e_w_gate": nrm(ks[17], (DEPTH, N_EXPERTS, D, D_EXPERT), D),
        "e_w_up": nrm(ks[18], (DEPTH, N_EXPERTS, D, D_EXPERT), D),
        "e_w_down": nrm(ks[19], (DEPTH, N_EXPERTS, D_EXPERT, D), D_EXPERT),
        "final_norm": gain(ks[20], (D,)),
    }


def reference(x, c, norm_mix, norm_ffn, w_ada, b_ada, a_w_qkv, a_w_o, b_w_in, b_v_gain, b_w_s, b_b_s, b_w_o,
              r_w_group, r_b_group, r_w_expert, r_b_expert, e_w_gate, e_w_up, e_w_down, final_norm):
    c_act = jax.nn.silu(c)
    for l in range(DEPTH):
        mod = c_act @ w_ada[l] + b_ada[l]
        sh_m, sc_m, gt_m, sh_f, sc_f, gt_f = jnp.split(mod, 6, axis=-1)
        h = modulate(rms_norm(x, norm_mix[l]), sh_m, sc_m)
        j = l // 2
        if l % 2 == 0:
            y = mixer_dilated_attention(h, a_w_qkv[j], a_w_o[j])
        else:
            y = mixer_spatial_gating(h, b_w_in[j], b_v_gain[j], b_w_s[j], b_b_s[j], b_w_o[j])
        x = x + (gt_m[:, None, :] * y).astype(x.dtype)
        h = modulate(rms_norm(x, norm_ffn[l]), sh_f, sc_f)
        y = hierarchical_moe(h, r_w_group[l], r_b_group[l], r_w_expert[l], r_b_expert[l],
                             e_w_gate[l], e_w_up[l], e_w_down[l])
        x = x + (gt_f[:, None, :] * y).astype(x.dtype)
    return rms_norm(x, final_norm)
```

```python
from contextlib import ExitStack

import numpy as np
import ml_dtypes

import concourse.bass as bass
import concourse.mybir as mybir
from concourse.bass_utils import run_bass_kernel_spmd

F32 = mybir.dt.float32
BF16 = mybir.dt.bfloat16
AF = mybir.ActivationFunctionType
ALU = mybir.AluOpType
AX = mybir.AxisListType

ENGS = ("pe", "act", "dve", "pool", "sp")
EPOCH = 30000
NDSEM = 12

SEQ = 16384
DM = 1024
TOWN = 4096
HALO = 1024
TEXT = TOWN + 2 * HALO
NCORES = 8
EPS = 1e-6
BIG = 1.0e4
SCRATCH_EXTERNAL = True


import types


def _freeze(fn):
    if fn is None or fn.__closure__ is None:
        return fn
    cells = []
    for c in fn.__closure__:
        try:
            cells.append(types.CellType(c.cell_contents))
        except ValueError:
            cells.append(c)
    return types.FunctionType(fn.__code__, fn.__globals__, fn.__name__, fn.__defaults__, tuple(cells))


class Reg:
    __slots__ = ("name", "w", "r", "excl")

    def __init__(self, name=""):
        self.name = name
        self.w = {}
        self.r = {}
        self.excl = name.startswith("p")


class Sched:
    def __init__(self, nc, stack):
        self.nc = nc
        self.stack = stack
        self.q = {e: [] for e in ENGS}
        self.cnt = {e: 0 for e in ENGS}
        self.epoch = {e: 0 for e in ENGS}
        self.sem = {}
        self.seen = {e: {} for e in ENGS}
        self.dsem = {}
        self.dcnt = {}
        self.bg = set()
        self.drr = {e: 0 for e in ENGS}
        for e in ENGS:
            self._newsem(e)

    def _newsem(self, e):
        k = (e, self.epoch[e])
        self.sem[k] = self.stack.enter_context(self.nc.semaphore(f"s_{e}_{self.epoch[e]}"))
        self.cnt[e] = 0

    def _handle(self, key):
        return self.dsem[key] if key[0] == "d" else self.sem[key]

    def _waits(self, e, deps):
        need = {}
        for t in deps:
            key, val = t
            if e == "pe" and key[0] == "pe":
                continue
            if self.seen[e].get(key, 0) >= val:
                continue
            if need.get(key, 0) < val:
                need[key] = val
        out = []
        for key, val in need.items():
            self.seen[e][key] = val
            out.append((self._handle(key), val))
        return out

    def _deps(self, reads, writes, extra, e=None):
        deps = list(extra)
        for r in reads:
            deps.extend(r.w.items())
            if r.excl:
                deps.extend((k, v) for k, v in r.r.items() if k[0] != e)
        for w in writes:
            deps.extend(w.w.items())
            deps.extend(w.r.items())
        return deps

    def _mark(self, tick, reads, writes):
        key, val = tick
        for r in reads:
            if r.r.get(key, 0) < val:
                r.r[key] = val
        for w in writes:
            w.w = {key: val}
            w.r = {}

    def op(self, e, fn, reads=(), writes=(), sig=True, extra=()):
        waits = self._waits(e, self._deps(reads, writes, extra, e))
        if sig:
            if self.cnt[e] >= EPOCH:
                self.epoch[e] += 1
                self._newsem(e)
            self.cnt[e] += 1
            key = (e, self.epoch[e])
            tick = (key, self.cnt[e])
        else:
            key = (e, self.epoch[e])
            tick = (key, self.cnt[e] + 1)
        self.q[e].append((waits, _freeze(fn), (self.sem[key], 1) if sig else None))
        self._mark(tick, reads, writes)
        return tick

    def dma(self, e, fn, reads=(), writes=(), extra=(), bg=False):
        waits = self._waits(e, self._deps(reads, writes, extra, e))
        anchor = writes[0] if writes else reads[0]
        key = ("d", id(anchor))
        if key not in self.dsem:
            self.dsem[key] = self.stack.enter_context(self.nc.semaphore(f"dq{len(self.dsem)}"))
            self.dcnt[key] = 0
        if bg:
            self.bg.add(key)
        self.dcnt[key] += 16
        tick = (key, self.dcnt[key])
        self.q[e].append((waits, _freeze(fn), (self.dsem[key], 16)))
        self._mark(tick, reads, writes)
        return tick

    def barrier(self, final=False):
        deps = []
        for e in ENGS:
            if self.cnt[e] > 0:
                deps.append(((e, self.epoch[e]), self.cnt[e]))
        for k, v in self.dcnt.items():
            if v > 0 and (final or k not in self.bg):
                deps.append((k, v))
        for e in ENGS:
            waits = self._waits(e, deps)
            if waits:
                self.q[e].append((waits, None, None))

    def emit(self):
        nc = self.nc
        engmap = {"pe": "tensor", "act": "scalar", "dve": "vector", "pool": "gpsimd", "sp": "sync"}
        with nc.Block() as block:
            for e in ENGS:
                items = self.q[e]
                if not items:
                    continue

                def body(eng, items=items):
                    for waits, fn, sig in items:
                        for h, v in waits:
                            eng.wait_ge(h, v)
                        if fn is None:
                            continue
                        ins = fn(eng)
                        if sig is not None:
                            ins.then_inc(sig[0], sig[1])

                getattr(block, engmap[e])(body)
        self.q = {e: [] for e in ENGS}


def build_program(nphase=5, dbg=False, asub=3):
    nc = bass.Bass("TRN2", target_bir_lowering=False)

    def din(name, shape, dt=F32):
        return nc.dram_tensor(name, list(shape), dt, kind="ExternalInput").ap()

    def dscr(name, shape, dt):
        return nc.dram_tensor(name, list(shape), dt, kind="Internal").ap()

    xext = din("xext", [TEXT, DM])
    cvec = din("cvec", [128, 8])
    w_ada = din("w_ada", [2, DM, 6 * DM])
    b_ada_l = din("b_ada_l", [128, 96])
    b_ada_r = din("b_ada_r", [2, 6 * DM])
    nrm = din("nrm", [128, 40])
    fin_row = din("fin_row", [1, DM])
    wqkv = din("wqkv", [DM, 2880])
    wo = din("wo", [960, DM])
    win = din("win", [DM, 4096])
    vgain = din("vgain", [1, 2048])
    ws = din("ws", [8, 128, 128])
    bsr = din("bsr", [1, 1024])
    bwo = din("bwo", [2048, DM])
    wr = din("wr", [2, DM, 20])
    brr = din("brr", [1, 40])
    eg = din("eg", [2, 16, DM, 256])
    eu = din("eu", [2, 16, DM, 256])
    ed = din("ed", [2, 16, 256, DM])
    cs = din("cs", [TEXT, 16])
    kval = din("kval", [128, 48])
    ident_d = din("ident", [128, 128], BF16)
    band_d = din("band", [128, 256], BF16)
    ones_d = din("ones", [1, 128], BF16)
    maskA_d = din("maskA", [128, DM])
    maskB_d = din("maskB", [128, DM])
    nrm2_d = din("nrm2", [128, 16])

    out_d = nc.dram_tensor("out", [TOWN, DM], F32, kind="ExternalOutput").ap()
    resid = nc.dram_tensor("resid", [TOWN, DM], F32, kind="ExternalOutput" if (dbg or SCRATCH_EXTERNAL) else "Internal").ap()

    wqkv_b = dscr("wqkv_b", [DM, 2880], BF16)
    wo_b = dscr("wo_b", [960, DM], BF16)
    win_b = dscr("win_b", [DM, 4096], BF16)
    bwo_b = dscr("bwo_b", [2048, DM], BF16)
    eg_b = dscr("eg_b", [2, 16, DM, 256], BF16)
    eu_b = dscr("eu_b", [2, 16, DM, 256], BF16)
    ed_b = dscr("ed_b", [2, 16, 256, DM], BF16)
    def dscr2(name, shape, dt):
        return nc.dram_tensor(name, list(shape), dt, kind="ExternalOutput" if (dbg or SCRATCH_EXTERNAL) else "Internal").ap()
    q_s = dscr2("q_s", [TOWN, 960], BF16)
    k_s = dscr2("k_s", [TEXT, 960], BF16)
    v_s = dscr2("v_s", [TEXT, 975], BF16)
    o_s = dscr2("o_s", [TOWN, 975], F32)

    with ExitStack() as gst:
        S = Sched(nc, gst)

        uid = [0]

        def mkTP(st):
            uid[0] += 1
            tag = f"ph{uid[0]}_"

            def T(name, shape, dt):
                return st.enter_context(nc.sbuf_tensor(tag + name, list(shape), dt))

            def P(name, shape, dt):
                return st.enter_context(nc.psum_tensor(tag + name, list(shape), dt))
            return T, P

        def GT(name, shape, dt):
            return gst.enter_context(nc.sbuf_tensor("g_" + name, list(shape), dt))

        ident = GT("ident_sb", [128, 128], BF16)
        modv = GT("modv", [128, 96], F32)
        Av = GT("Av", [128, 32], F32)
        modv2 = GT("modv2", [128, 32], F32)
        nrm_sb = GT("nrm_sb", [128, 40], F32)
        gtbc = GT("gtbc", [128, 4, DM], F32)
        finbc = GT("finbc", [128, DM], F32)
        R_const = Reg("const")
        R_mod = Reg("mod")
        R_gtbc = Reg("gtbc")
        R_w = {n: Reg(n) for n in ("wqkv_b", "wo_b", "win_b", "bwo_b", "eg0", "eu0", "ed0", "eg1", "eu1", "ed1")}
        R_q, R_k, R_v, R_o = Reg("q_s"), Reg("k_s"), Reg("v_s"), Reg("o_s")
        R_res = [Reg(f"res{t}") for t in range(8)]
        R_out = Reg("out")

        def sh_of(l, which):
            return l * 48 + (0 if which == "m" else 24)

        def gt_idx(l, which):
            return l * 2 + (0 if which == "m" else 1)

        def issue_big_casts():
            for l in range(2):
                for nm, src, dst in (("eg", eg, eg_b), ("eu", eu, eu_b), ("ed", ed, ed_b)):
                    S.dma("pool", lambda e: e.dma_start(
                        out=dst[l].rearrange("e a b -> (e a) b"), in_=src[l].rearrange("e a b -> (e a) b")),
                        writes=[R_w[f"{nm}{l}"]], bg=True)
                if l == 0:
                    S.dma("pool", lambda e: e.dma_start(out=win_b, in_=win), writes=[R_w["win_b"]], bg=True)
                    S.dma("pool", lambda e: e.dma_start(out=bwo_b, in_=bwo), writes=[R_w["bwo_b"]], bg=True)

        def adaln_tasks(T, pbc, R_pbc, vectors):
            cv = T("cv", [128, 8], F32)
            cact = T("cact", [128, 8], F32)
            cbc = T("cbc", [128, 8, 128], F32)
            maskA = T("maskA", [128, DM], F32)
            maskB = T("maskB", [128, DM], F32)
            nrm2 = T("nrm2", [128, 16], F32)
            mvec = [T(f"mvec{i}", [128, DM], F32) for i in range(2)]
            bro = [T(f"bro{i}", [128, 512], F32) for i in range(2)]
            wa = [T(f"wa{i}", [128, 8, 512], F32) for i in range(2)]
            xtmp = T("xtmp", [128, DM], F32)
            sct = T("sct", [128, 8], F32)
            R_cv, R_cact, R_cbc, R_msk, R_sct, R_xtmp = (Reg(n) for n in ("cv", "cact", "cbc", "msk", "sct", "xtmp"))
            R_wa = [Reg("wa0"), Reg("wa1")]
            R_bro = [Reg("bro0"), Reg("bro1")]
            R_mvec = [Reg("mvec0"), Reg("mvec1")]

            def setup():
                S.dma("sp", lambda e: e.dma_start(out=cv[:], in_=cvec), writes=[R_cv])
                S.dma("sp", lambda e: e.dma_start(out=maskA[:], in_=maskA_d), writes=[R_msk])
                S.dma("sp", lambda e: e.dma_start(out=maskB[:], in_=maskB_d), writes=[R_msk])
                S.dma("sp", lambda e: e.dma_start(out=nrm2[:], in_=nrm2_d), writes=[R_msk])
                S.op("act", lambda e: e.activation(out=cact[:], in_=cv[:], func=AF.Silu), reads=[R_cv], writes=[R_cact])
                S.op("dve", lambda e: e.tensor_copy(out=cbc[:], in_=cact[:].unsqueeze(2).to_broadcast([128, 8, 128])),
                     reads=[R_cact], writes=[R_cbc])

            def extract(dst, R_dst, src, R_src, perm):
                msk = maskB if perm else maskA
                S.op("pool", lambda e: e.tensor_tensor(out=xtmp[:], in0=src[:], in1=msk[:], op=ALU.mult),
                     reads=[R_src, R_msk], writes=[R_xtmp])
                view = (xtmp[:].rearrange("p (q k) -> p k q", k=8) if perm else xtmp[:].rearrange("p (j q) -> p j q", q=128))
                S.op("dve", lambda e: e.tensor_reduce(out=dst.unsqueeze(2), in_=view, axis=AX.X, op=ALU.add),
                     reads=[R_xtmp], writes=[R_dst])

            def consume(l, idx, mv, R_mv):
                if idx == 0:
                    extract(modv[:, l * 48:l * 48 + 8], R_mod, mv, R_mv, False)
                elif idx == 1:
                    extract(sct[:], R_sct, mv, R_mv, False)
                    S.op("dve", lambda e: e.scalar_tensor_tensor(
                        out=Av[:, l * 8:(l + 1) * 8], in0=sct[:], scalar=1.0, in1=nrm_sb[:, l * 8:(l + 1) * 8],
                        op0=ALU.add, op1=ALU.mult), reads=[R_sct, R_const], writes=[R_mod])
                elif idx == 3:
                    extract(modv2[:, l * 16:l * 16 + 8], R_mod, mv, R_mv, True)
                elif idx == 4:
                    extract(sct[:], R_sct, mv, R_mv, True)
                    S.op("dve", lambda e: e.scalar_tensor_tensor(
                        out=modv2[:, l * 16 + 8:l * 16 + 16], in0=sct[:], scalar=1.0, in1=nrm2[:, l * 8:(l + 1) * 8],
                        op0=ALU.add, op1=ALU.mult), reads=[R_sct, R_msk], writes=[R_mod])
                else:
                    gi = gt_idx(l, "m" if idx == 2 else "f")
                    S.op("act", lambda e: e.copy(out=gtbc[:, gi, :], in_=mv[:]), reads=[R_mv], writes=[R_gtbc])

            tasks = [setup]
            cnt_b = [0]
            for vi, (l, idx) in enumerate(vectors):
                for half in range(2):
                    def task(l=l, idx=idx, half=half, vi=vi):
                        wb = cnt_b[0] % 2
                        cnt_b[0] += 1
                        mi = vi % 2
                        c0 = idx * DM + half * 512
                        S.dma("sp", lambda e: e.dma_start(
                            out=wa[wb][:], in_=w_ada[l, :, c0:c0 + 512].rearrange("(k p) n -> p k n", p=128)), writes=[R_wa[wb]])
                        S.dma("sp", lambda e: e.dma_start(
                            out=bro[wb][:], in_=b_ada_r[l:l + 1, c0:c0 + 512].partition_broadcast(128)), writes=[R_bro[wb]])
                        pb = wb % len(pbc)
                        for k in range(8):
                            S.op("pe", lambda e: e.matmul(
                                pbc[pb][:], lhsT=cbc[:, k, :], rhs=wa[wb][:, k, :], start=(k == 0), stop=(k == 7)),
                                reads=[R_wa[wb], R_cbc], writes=[R_pbc[pb]] if k == 0 else [], sig=(k == 7))
                        S.op("dve", lambda e: e.tensor_tensor(
                            out=mvec[mi][:, half * 512:(half + 1) * 512], in0=pbc[pb][:], in1=bro[wb][:], op=ALU.add),
                            reads=[R_pbc[pb], R_bro[wb]], writes=[R_mvec[mi]])
                        if half == 1:
                            consume(l, idx, mvec[mi], R_mvec[mi])
                    tasks.append(task)
            return tasks

        with ExitStack() as st:
            T, P = mkTP(st)

            S.dma("pool", lambda e: e.dma_start(out=wqkv_b, in_=wqkv), writes=[R_w["wqkv_b"]], bg=True)
            S.dma("pool", lambda e: e.dma_start(out=wo_b, in_=wo), writes=[R_w["wo_b"]], bg=True)
            S.dma("sp", lambda e: e.dma_start(out=ident[:], in_=ident_d), writes=[R_const])
            S.dma("sp", lambda e: e.dma_start(out=nrm_sb[:], in_=nrm), writes=[R_const])
            S.dma("sp", lambda e: e.dma_start(out=finbc[:], in_=fin_row.partition_broadcast(128)), writes=[R_const])

            pbc0 = [P(f"pbc{i}", [128, 512], F32) for i in range(2)]
            for tk in adaln_tasks(T, pbc0, [Reg("pbc0"), Reg("pbc1")], [(0, 0), (0, 1)]):
                tk()
            if dbg:
                d_modv = nc.dram_tensor("d_modv", [128, 96], F32, kind="ExternalOutput").ap()
                d_av = nc.dram_tensor("d_av", [128, 32], F32, kind="ExternalOutput").ap()
                d_gtbc = nc.dram_tensor("d_gtbc", [128, 4 * DM], F32, kind="ExternalOutput").ap()
                S.dma("sp", lambda e: e.dma_start(out=d_modv, in_=modv[:]), reads=[R_mod])
                S.dma("sp", lambda e: e.dma_start(out=d_av, in_=modv2[:]), reads=[R_mod])
                S.dma("sp", lambda e: e.dma_start(out=d_gtbc, in_=gtbc[:].rearrange("p a b -> p (a b)")), reads=[R_gtbc])
            S.barrier()
            S.emit()

        def A_of(l, which):
            i = (0 if which == "m" else 2) + l
            return i * 8

        def norm_transpose(xin, R_xin, xn, R_xn, hT, R_hT, pT, R_pT, junk, R_junk, ss, rstd, R_ss, a_ap, s_ap, perm=False):
            for s in range(4):
                S.op("act", lambda e, s=s: e.activation(out=junk[:], in_=xin[:, s, :], func=AF.Square, accum_out=ss[:, s:s + 1]),
                     reads=[R_xin], writes=[R_junk, R_ss])
            S.op("act", lambda e: e.activation(out=rstd[:], in_=ss[:], func=AF.Sqrt, scale=1.0 / DM, bias=EPS),
                 reads=[R_ss], writes=[R_ss])
            S.op("dve", lambda e: e.reciprocal(out=rstd[:], in_=rstd[:]), reads=[R_ss], writes=[R_ss])
            for s in range(4):
                if s % 2 == 0:
                    S.op("dve", lambda e, s=s: e.tensor_scalar(out=xn[:, s, :], in0=xin[:, s, :], scalar1=rstd[:, s:s + 1],
                                                                scalar2=None, op0=ALU.mult), reads=[R_xin, R_ss], writes=[R_xn])
                else:
                    S.op("act", lambda e, s=s: e.activation(out=xn[:, s, :], in_=xin[:, s, :], func=AF.Identity,
                                                             scale=rstd[:, s:s + 1]), reads=[R_xin, R_ss], writes=[R_xn])
            for k in range(8):
                pb = k % 2
                for s in range(4):
                    src = (xn[:, s, :].rearrange("p (q k) -> p k q", k=8)[:, k, :] if perm else xn[:, s, k * 128:(k + 1) * 128])
                    S.op("pe", lambda e, k=k, s=s, pb=pb, src=src: e.transpose(pT[pb][:, s * 128:(s + 1) * 128], src, ident[:]),
                         reads=[R_xn, R_const], writes=[R_pT[pb]] if s == 0 else [], sig=(s == 3))
                S.op("act", lambda e, k=k, pb=pb: e.activation(out=hT[:, k, :], in_=pT[pb][:, 0:512], func=AF.Identity,
                                                                scale=a_ap[:, k:k + 1], bias=s_ap[:, k:k + 1]),
                     reads=[R_pT[pb], R_mod], writes=[R_hT])

        if nphase >= 1:
            with ExitStack() as st:
                T, P = mkTP(st)

                issue_big_casts()
                wq = T("wq", [128, 8, 2880], BF16)
                cs_sb = T("cs_sb", [128, 48, 16], F32)
                kv_sb = T("kv_sb", [128, 48], F32)
                xin = [T(f"xin{i}", [128, 4, DM], F32) for i in range(2)]
                xn = T("xn", [128, 4, DM], BF16)
                hT = [T(f"hT{i}", [128, 8, 512], BF16) for i in range(2)]
                junk = T("junk", [128, DM], BF16)
                ss = [T(f"ss{i}", [128, 4], F32) for i in range(2)]
                rstd = [T(f"rstd{i}", [128, 4], F32) for i in range(2)]
                qst = [T(f"qst{i}", [128, 960], BF16) for i in range(2)]
                kst = [T(f"kst{i}", [128, 960], BF16) for i in range(2)]
                vst = [T(f"vst{i}", [128, 975], BF16) for i in range(2)]
                tr = [T(f"tr{i}", [128, 4, 8, 8], F32) for i in range(2)]
                pT = [P(f"pT{i}", [128, 1024], BF16) for i in range(2)]
                pb = [P(f"pb{i}", [128, 512], F32) for i in range(6)]
                R_wq, R_cs = Reg("wq"), Reg("cs")
                R_xin = [Reg("xin0"), Reg("xin1")]
                R_xn = Reg("xn")
                R_hT = [Reg("hT0"), Reg("hT1")]
                R_junk = Reg("junk")
                R_ss = [Reg("ss0"), Reg("ss1")]
                R_qst = [Reg("qst0"), Reg("qst1")]
                R_kst = [Reg("kst0"), Reg("kst1")]
                R_vst = [Reg("vst0"), Reg("vst1")]
                R_tr = [Reg("tr0"), Reg("tr1")]
                R_pT = [Reg("pT0"), Reg("pT1")]
                R_pb = [Reg(f"pb{i}") for i in range(6)]

                S.dma("sp", lambda e: e.dma_start(out=cs_sb[:], in_=cs.rearrange("(t p) c -> p t c", p=128)), writes=[R_cs])
                S.dma("sp", lambda e: e.dma_start(out=kv_sb[:], in_=kval), writes=[R_cs])
                for k in range(8):
                    S.dma("sp", lambda e, k=k: e.dma_start(out=wq[:, k, :], in_=wqkv_b[k * 128:(k + 1) * 128, :]),
                          reads=[R_w["wqkv_b"]], writes=[R_wq])

                a0, s0 = A_of(0, "m"), sh_of(0, "m")
                trc = 0
                stc = 0
                import os
                CUT = int(os.environ.get("A1CUT", "9"))
                SKIP = os.environ.get("A1SKIP", "")
                def a1_load(tg):
                    xb = tg % 2
                    S.dma("sp", lambda e: e.dma_start(
                        out=xin[xb][:], in_=xext[tg * 512:(tg + 1) * 512, :].rearrange("(s p) d -> p s d", p=128)),
                        writes=[R_xin[xb]])

                def a1_norm(tg):
                    xb = tg % 2
                    norm_transpose(xin[xb], R_xin[xb], xn, R_xn, hT[xb], R_hT[xb], pT, R_pT, junk, R_junk,
                                   ss[xb], rstd[xb], R_ss[xb], Av[:, 0:8], modv[:, 0:8])

                a1_load(0)
                a1_load(1)
                a1_norm(0)
                for tg in range(12):
                    own = 2 <= tg < 10
                    xb = tg % 2
                    if tg + 2 < 12:
                        a1_load(tg + 2)
                    if tg + 1 < 12:
                        a1_norm(tg + 1)
                    for s in range(4):
                        if CUT < 2:
                            break
                        it = tg * 4 + s
                        sb = stc % 2
                        stc += 1
                        blocks = []
                        if own:
                            blocks += [("q", 0, 512, 0), ("q", 512, 960, 1)]
                        blocks += [("k", 960, 1472, 2), ("k", 1472, 1920, 3), ("v", 1920, 2432, 4), ("v", 2432, 2880, 5)]
                        for kind, c0, c1, bi in blocks:
                            n = c1 - c0
                            for k in range(8):
                                S.op("pe", lambda e, k=k, s=s, c0=c0, c1=c1, n=n, bi=bi, xb=xb: e.matmul(
                                    pb[bi][:, 0:n], lhsT=hT[xb][:, k, s * 128:(s + 1) * 128], rhs=wq[:, k, c0:c1],
                                    start=(k == 0), stop=(k == 7)),
                                    reads=[R_hT[xb], R_wq], writes=[R_pb[bi]] if k == 0 else [], sig=(k == 7))
                            nh = n // 64
                            if CUT < 3:
                                continue
                            if kind in ("q", "k"):
                                stg = qst[sb] if kind == "q" else kst[sb]
                                R_stg = R_qst[sb] if kind == "q" else R_kst[sb]
                                base = c0 if kind == "q" else c0 - 960
                                pv = pb[bi][:, 0:n].rearrange("p (h c) -> p h c", c=64)
                                sv = stg[:, base:base + n].rearrange("p (h c) -> p h c", c=64)
                                ti = trc % 2
                                trc += 1
                                cosb = cs_sb[:, it, 0:8].unsqueeze(1).to_broadcast([128, nh, 8])
                                sinb = cs_sb[:, it, 8:16].unsqueeze(1).to_broadcast([128, nh, 8])
                                if "a" not in SKIP:
                                    S.op("act", lambda e, pv=pv, sv=sv: e.copy(out=sv[:, :, 16:64], in_=pv[:, :, 16:64]),
                                         reads=[R_pb[bi]], writes=[R_stg])
                                for ti2, (a, b) in enumerate((((0, 8), cosb), ((8, 16), sinb), ((8, 16), cosb), ((0, 8), sinb))):
                                    if "b" in SKIP:
                                        continue
                                    S.op("dve", lambda e, pv=pv, a=a, b=b, ti=ti, ti2=ti2, nh=nh: e.tensor_tensor(
                                        out=tr[ti][:, ti2, 0:nh, :], in0=pv[:, :, a[0]:a[1]], in1=b, op=ALU.mult),
                                        reads=[R_pb[bi], R_cs], writes=[R_tr[ti]])
                                if "c" not in SKIP:
                                  S.op("pool", lambda e, sv=sv, ti=ti, nh=nh: e.tensor_tensor(
                                    out=sv[:, :, 0:8], in0=tr[ti][:, 0, 0:nh, :], in1=tr[ti][:, 1, 0:nh, :], op=ALU.subtract),
                                    reads=[R_tr[ti]], writes=[R_stg])
                                if "c" not in SKIP:
                                  S.op("pool", lambda e, sv=sv, ti=ti, nh=nh: e.tensor_tensor(
                                    out=sv[:, :, 8:16], in0=tr[ti][:, 2, 0:nh, :], in1=tr[ti][:, 3, 0:nh, :], op=ALU.add),
                                    reads=[R_tr[ti]], writes=[R_stg])
                            else:
                                h0 = (c0 - 1920) // 64
                                pv = pb[bi][:, 0:n].rearrange("p (h c) -> p h c", c=64)
                                vv = vst[sb][:].rearrange("p (h c) -> p h c", c=65)
                                if "d" not in SKIP:
                                  S.op("act", lambda e, pv=pv, vv=vv, h0=h0, nh=nh, it=it: e.activation(
                                    out=vv[:, h0:h0 + nh, 0:64], in_=pv, func=AF.Identity, scale=kv_sb[:, it:it + 1]),
                                    reads=[R_pb[bi], R_cs], writes=[R_vst[sb]])
                                if bi == 5 and "e" not in SKIP:
                                    S.op("pool", lambda e, vv=vv, it=it: e.tensor_copy(
                                        out=vv[:, :, 64:65], in_=kv_sb[:, it:it + 1].unsqueeze(1).to_broadcast([128, 15, 1])),
                                        reads=[R_cs], writes=[R_vst[sb]])
                        if CUT < 4:
                            continue
                        if own:
                            ot = it - 8
                            S.dma("sp", lambda e, ot=ot, sb=sb: e.dma_start(out=q_s[ot * 128:(ot + 1) * 128, :], in_=qst[sb][:]),
                                  reads=[R_qst[sb]], writes=[])
                        S.dma("sp", lambda e, it=it, sb=sb: e.dma_start(out=k_s[it * 128:(it + 1) * 128, :], in_=kst[sb][:]),
                              reads=[R_kst[sb]], writes=[])
                        S.dma("sp", lambda e, it=it, sb=sb: e.dma_start(out=v_s[it * 128:(it + 1) * 128, :], in_=vst[sb][:]),
                              reads=[R_vst[sb]], writes=[])
                S.barrier()
                S.emit()

        if nphase >= 1 and asub >= 2:
            with ExitStack() as st:
                T, P = mkTP(st)

                UB = 8
                band = T("band", [128, 256], BF16)
                Qt = [T(f"Qt{i}", [128, UB, 384], BF16) for i in range(2)]
                Kt = [T(f"Kt{i}", [128, UB + 1, 384], BF16) for i in range(2)]
                Vt = [T(f"Vt{i}", [128, UB + 1, 325], BF16) for i in range(2)]
                qT = [T(f"qT{i}", [128, 3, UB * 128], BF16) for i in range(2)]
                kT = [T(f"kT{i}", [128, 3, (UB + 1) * 128], BF16) for i in range(2)]
                NPS = 3
                Pe = [T(f"Pe{i}", [128, 5, 256], BF16) for i in range(NPS)]
                PT = [T(f"PT{i}", [128, 5, 256], BF16) for i in range(NPS)]
                Oev = [T(f"Oev{i}", [128, 325], F32) for i in range(3)]
                NSB = 4
                pTr1 = P("pTr0", [128, 1024], BF16)
                pTr = [pTr1, pTr1]
                pbcA = P("pbcA", [128, 512], F32)
                pS = [P(f"pS{i}", [128, 512], F32) for i in range(NSB)]
                pO = [P(f"pO{i}", [128, 512], F32) for i in range(2)]
                R_band = Reg("band")
                R_Qt = [Reg("Qt0"), Reg("Qt1")]
                R_Kt = [Reg("Kt0"), Reg("Kt1")]
                R_Vt = [Reg("Vt0"), Reg("Vt1")]
                R_qT = [Reg("qT0"), Reg("qT1")]
                R_kT = [Reg("kT0"), Reg("kT1")]
                R_Pe = [Reg(f"Pe{i}") for i in range(NPS)]
                R_PT = [Reg(f"PT{i}") for i in range(NPS)]
                R_Oev = [Reg(f"Oev{i}") for i in range(3)]
                R_pTr = [Reg("pTr0")] * 2
                R_pS = [Reg(f"pS{i}") for i in range(NSB)]
                ada_rest = adaln_tasks(T, [pbcA], [Reg("pbcA")],
                                       [(0, 2), (0, 3), (0, 4), (0, 5), (1, 0), (1, 1), (1, 2), (1, 3), (1, 4), (1, 5)])
                R_pO = [Reg("pO0"), Reg("pO1")]
                S.dma("sp", lambda e: e.dma_start(out=band[:], in_=band_d), writes=[R_band])
                for i in range(2):
                    S.op("pool", lambda e, i=i: e.memset(Qt[i][:, :, 320:384], 0.0), writes=[R_Qt[i]])
                    S.op("pool", lambda e, i=i: e.memset(Kt[i][:, :, 320:384], 0.0), writes=[R_Kt[i]])

                units = []
                for g, d in enumerate((1, 4, 16)):
                    nbt = 32 // d
                    ub = min(UB, nbt)
                    for r in range(d):
                        for n0 in range(0, nbt, ub):
                            units.append((g, d, r, n0, ub))

                def load_qk(u, ub_i):
                    g, d, r, n0, nb = u
                    qv = q_s.rearrange("(i d) c -> d i c", d=d)
                    kv = k_s.rearrange("(i d) c -> d i c", d=d)
                    i0 = n0 * 128 - 64 + HALO // d
                    S.dma("sp", lambda e: e.dma_start(
                        out=Qt[ub_i][:, 0:nb, 0:320],
                        in_=qv[r, n0 * 128:(n0 + nb) * 128, g * 320:(g + 1) * 320].rearrange("(n p) c -> p n c", p=128)),
                        reads=[R_q], writes=[R_Qt[ub_i]])
                    S.dma("sp", lambda e: e.dma_start(
                        out=Kt[ub_i][:, 0:nb + 1, 0:320],
                        in_=kv[r, i0:i0 + (nb + 1) * 128, g * 320:(g + 1) * 320].rearrange("(n p) c -> p n c", p=128)),
                        reads=[R_k], writes=[R_Kt[ub_i]])

                def load_v(u, ub_i):
                    g, d, r, n0, nb = u
                    vv = v_s.rearrange("(i d) c -> d i c", d=d)
                    i0 = n0 * 128 - 64 + HALO // d
                    S.dma("sp", lambda e: e.dma_start(
                        out=Vt[ub_i][:, 0:nb + 1, :],
                        in_=vv[r, i0:i0 + (nb + 1) * 128, g * 325:(g + 1) * 325].rearrange("(n p) c -> p n c", p=128)),
                        reads=[R_v], writes=[R_Vt[ub_i]])

                cnt = {"tr": 0, "ps": 0, "pe": 0, "oe": 0}

                def make_tasks(u, bi):
                    g, d, r, n0, nb = u
                    tasks = []
                    for src, R_src, dst, R_dst, cnt_n in ((Qt[bi], R_Qt[bi], qT[bi], R_qT[bi], nb),
                                                            (Kt[bi], R_Kt[bi], kT[bi], R_kT[bi], nb + 1)):
                        for n in range(cnt_n):
                            def task(src=src, R_src=R_src, dst=dst, R_dst=R_dst, n=n):
                                tb = cnt["tr"] % 2
                                cnt["tr"] += 1
                                for pr in range(3):
                                    S.op("pe", lambda e: e.transpose(
                                        pTr[tb][:, pr * 128:(pr + 1) * 128], src[:, n, pr * 128:(pr + 1) * 128], ident[:]),
                                        reads=[R_src, R_const], writes=[R_pTr[tb]] if pr == 0 else [], sig=(pr == 2))
                                if tb == 0:
                                    S.op("act", lambda e: e.copy(
                                        out=dst[:, 0:2, n * 128:(n + 1) * 128],
                                        in_=pTr[tb][:, 0:256].rearrange("p (a c) -> p a c", c=128)), reads=[R_pTr[tb]], writes=[R_dst])
                                    S.op("act", lambda e: e.copy(
                                        out=dst[0:64, 2, n * 128:(n + 1) * 128], in_=pTr[tb][0:64, 256:384]),
                                        reads=[R_pTr[tb]], writes=[R_dst])
                                else:
                                    S.op("dve", lambda e: e.tensor_copy(
                                        out=dst[:, 0:2, n * 128:(n + 1) * 128],
                                        in_=pTr[tb][:, 0:256].rearrange("p (a c) -> p a c", c=128)), reads=[R_pTr[tb]], writes=[R_dst])
                                    S.op("dve", lambda e: e.tensor_copy(
                                        out=dst[0:64, 2, n * 128:(n + 1) * 128], in_=pTr[tb][0:64, 256:384]),
                                        reads=[R_pTr[tb]], writes=[R_dst])
                            tasks.append(task)
                    return tasks

                def s_stage(u, bi, j):
                    g, d, r, n0, nb = u
                    c0 = 0 if j >= 1 else 128
                    c1 = 256 if j < nb else 128
                    q0 = (j - 1) * 128 + c0
                    wq_ = c1 - c0
                    pi = cnt["pe"] % NPS
                    cnt["pe"] += 1
                    for bk, hs in enumerate(((0, 2), (1, 3), (4,))):
                        sbk = cnt["ps"] % NSB
                        cnt["ps"] += 1
                        for hh, h in enumerate(hs):
                            pr, base = h // 2, (h % 2) * 64
                            S.op("pe", lambda e: e.matmul(
                                pS[sbk][:, hh * 256 + c0:hh * 256 + c1],
                                lhsT=kT[bi][base:base + 64, pr, j * 128:(j + 1) * 128],
                                rhs=qT[bi][base:base + 64, pr, q0:q0 + wq_], start=True, stop=True),
                                reads=[R_kT[bi], R_qT[bi]], writes=[R_pS[sbk]] if hh == 0 else [],
                                sig=(hh == len(hs) - 1))
                        nhb = len(hs)
                        S.op("act", lambda e: e.activation(
                            out=Pe[pi][:, bk * 2:bk * 2 + nhb, c0:c1],
                            in_=pS[sbk][:, 0:nhb * 256].rearrange("p (h c) -> p h c", c=256)[:, :, c0:c1],
                            func=AF.Exp, scale=0.125), reads=[R_pS[sbk]], writes=[R_Pe[pi]])
                    S.op("dve", lambda e: e.tensor_tensor(
                        out=PT[pi][:, :, c0:c1], in0=Pe[pi][:, :, c0:c1],
                        in1=band[:, c0:c1].unsqueeze(1).to_broadcast([128, 5, c1 - c0]), op=ALU.mult),
                        reads=[R_Pe[pi], R_band], writes=[R_PT[pi]])
                    return pi

                def pv_stage(u, bi, j, pi):
                    g, d, r, n0, nb = u
                    for n in (j - 1, j):
                        if n < 0 or n >= nb:
                            continue
                        cc = 0 if n == j - 1 else 128
                        ob = n % 2
                        for h in range(5):
                            sl = (0, 2, 1, 3, 4)[h]
                            S.op("pe", lambda e: e.matmul(
                                pO[ob][:, h * 65:(h + 1) * 65], lhsT=PT[pi][:, sl, cc:cc + 128],
                                rhs=Vt[bi][:, j, h * 65:(h + 1) * 65], start=(j == n and h == 0), stop=(j == n + 1),
                                skip_group_check=True),
                                reads=[R_PT[pi], R_Vt[bi]], writes=[R_pO[ob]] if h == 0 else [], sig=(h == 4))
                        if j == n + 1:
                            oi = cnt["oe"] % 3
                            cnt["oe"] += 1
                            if cnt["oe"] % 2 == 0:
                                S.op("act", lambda e: e.copy(out=Oev[oi][:], in_=pO[ob][:, 0:325]),
                                     reads=[R_pO[ob]], writes=[R_Oev[oi]])
                            else:
                                S.op("dve", lambda e: e.tensor_copy(out=Oev[oi][:], in_=pO[ob][:, 0:325]),
                                     reads=[R_pO[ob]], writes=[R_Oev[oi]])
                            ov = o_s.rearrange("(i d) c -> d i c", d=d)
                            nn = n0 + n
                            S.dma("sp", lambda e: e.dma_start(
                                out=ov[r, nn * 128:(nn + 1) * 128, g * 325:(g + 1) * 325], in_=Oev[oi][:]),
                                reads=[R_Oev[oi]], writes=[])

                NU = len(units)
                load_qk(units[0], 0)
                load_v(units[0], 0)
                if NU > 1:
                    load_qk(units[1], 1)
                for tk in make_tasks(units[0], 0):
                    tk()
                for ui, u in enumerate(units):
                    bi = ui % 2
                    nb = u[4]
                    if ui + 2 < NU:
                        load_qk(units[ui + 2], bi)
                    if ui + 1 < NU:
                        load_v(units[ui + 1], 1 - bi)
                    tasks = make_tasks(units[ui + 1], 1 - bi) if ui + 1 < NU else []
                    per = -(-len(tasks) // (nb + 1)) if tasks else 0
                    prev = None
                    for j in range(nb + 1):
                        pi = s_stage(u, bi, j)
                        if j == nb // 2 and ada_rest:
                            if ui == 0:
                                ada_rest.pop(0)()
                            ada_rest.pop(0)()
                        for _ in range(per):
                            if tasks:
                                tasks.pop(0)()
                        if prev is not None:
                            pv_stage(u, bi, prev[0], prev[1])
                        prev = (j, pi)
                    pv_stage(u, bi, prev[0], prev[1])
                    while tasks:
                        tasks.pop(0)()
                while ada_rest:
                    ada_rest.pop(0)()
                S.barrier()
                S.emit()

        if nphase >= 1 and asub >= 3:
            with ExitStack() as st:
                T, P = mkTP(st)

                wo_sb = T("wo_sb", [128, 8, DM], BF16)
                Ot = [T(f"Ot{i}", [128, 975], F32) for i in range(2)]
                xt = [T(f"xt{i}", [128, DM], F32) for i in range(3)]
                Dn = [T(f"Dn{i}", [128, 5], F32) for i in range(2)]
                ob = [T(f"ob{i}", [128, 1024], BF16) for i in range(2)]
                oT = [T(f"oT{i}", [128, 8, 128], BF16) for i in range(2)]
                tmp = [T(f"tmp{i}", [128, 512], F32) for i in range(2)]
                pTr = [P(f"pTr{i}", [128, 1024], BF16) for i in range(2)]
                py = [P(f"py{i}", [128, 512], F32) for i in range(4)]
                R_wo = Reg("wo")
                R_Ot = [Reg("Ot0"), Reg("Ot1")]
                R_xt = [Reg("xt0"), Reg("xt1"), Reg("xt2")]
                R_Dn = [Reg("Dn0"), Reg("Dn1")]
                R_ob = [Reg("ob0"), Reg("ob1")]
                R_oT = [Reg("oT0"), Reg("oT1")]
                R_tmp = [Reg("tmp0"), Reg("tmp1")]
                R_pTr = [Reg("pTr0"), Reg("pTr1")]
                R_py = [Reg(f"py{i}") for i in range(4)]
                S.dma("sp", lambda e: e.dma_start(out=wo_sb[:, 0:7, :], in_=wo_b[0:896, :].rearrange("(k p) n -> p k n", p=128)),
                      reads=[R_w["wo_b"]], writes=[R_wo])
                S.dma("sp", lambda e: e.dma_start(out=wo_sb[0:64, 7, :], in_=wo_b[896:960, :]), reads=[R_w["wo_b"]], writes=[R_wo])
                gi = gt_idx(0, "m")
                tmc = 0
                for i in range(2):
                    S.op("pool", lambda e, i=i: e.memset(ob[i][:, 960:1024], 0.0), writes=[R_ob[i]])
                tmc3 = [0]

                def a3_loads(t):
                    b = t % 2
                    xb3 = t % 3
                    S.dma("sp", lambda e: e.dma_start(out=Ot[b][:], in_=o_s[t * 128:(t + 1) * 128, :]),
                          reads=[R_o], writes=[R_Ot[b]])
                    S.dma("sp", lambda e: e.dma_start(out=xt[xb3][:], in_=xext[HALO + t * 128:HALO + (t + 1) * 128, :]),
                          writes=[R_xt[xb3]])

                def a3_stage1(t):
                    b = t % 2
                    O4 = Ot[b][:].rearrange("p (g h c) -> p g h c", g=3, h=5)
                    S.op("dve", lambda e, O4=O4, b=b: e.tensor_tensor(out=Dn[b][:], in0=O4[:, 0, :, 64], in1=O4[:, 1, :, 64], op=ALU.add),
                         reads=[R_Ot[b]], writes=[R_Dn[b]])
                    S.op("dve", lambda e, O4=O4, b=b: e.tensor_tensor(out=Dn[b][:], in0=Dn[b][:], in1=O4[:, 2, :, 64], op=ALU.add),
                         reads=[R_Ot[b], R_Dn[b]], writes=[R_Dn[b]])
                    S.op("dve", lambda e, b=b: e.reciprocal(out=Dn[b][:], in_=Dn[b][:]), reads=[R_Dn[b]], writes=[R_Dn[b]])
                    ob4 = ob[b][:, 0:960].rearrange("p (g h c) -> p g h c", g=3, h=5)
                    for g in range(3):
                        eng = "pool" if g < 2 else "dve"
                        S.op(eng, lambda e, g=g, O4=O4, ob4=ob4, b=b: e.tensor_tensor(
                            out=ob4[:, g], in0=O4[:, g, :, 0:64], in1=Dn[b][:].unsqueeze(2).to_broadcast([128, 5, 64]), op=ALU.mult),
                            reads=[R_Ot[b], R_Dn[b]], writes=[R_ob[b]])

                def a3_stage2(t):
                    b = t % 2
                    xb3 = t % 3
                    for kc in range(8):
                        w = 128
                        S.op("pe", lambda e, kc=kc, w=w, b=b: e.transpose(
                            pTr[b][0:w, kc * 128:(kc + 1) * 128], ob[b][:, kc * 128:kc * 128 + w], ident[:]),
                            reads=[R_ob[b], R_const], writes=[R_pTr[b]] if kc == 0 else [], sig=(kc == 7))
                    S.op("act", lambda e, b=b: e.copy(out=oT[b][:, 0:7, :], in_=pTr[b][:, 0:896].rearrange("p (k c) -> p k c", c=128)),
                         reads=[R_pTr[b]], writes=[R_oT[b]])
                    S.op("act", lambda e, b=b: e.copy(out=oT[b][0:64, 7, :], in_=pTr[b][0:64, 896:1024]),
                         reads=[R_pTr[b]], writes=[R_oT[b]])
                    for nbk in range(2):
                        yb = (t * 2 + nbk) % 4
                        for kc in range(8):
                            w = 128 if kc < 7 else 64
                            S.op("pe", lambda e, kc=kc, w=w, nbk=nbk, yb=yb, b=b: e.matmul(
                                py[yb][:], lhsT=oT[b][0:w, kc, :], rhs=wo_sb[0:w, kc, nbk * 512:(nbk + 1) * 512],
                                start=(kc == 0), stop=(kc == 7)),
                                reads=[R_oT[b], R_wo], writes=[R_py[yb]] if kc == 0 else [], sig=(kc == 7))
                        ti = tmc3[0] % 2
                        tmc3[0] += 1
                        S.op("dve", lambda e, yb=yb, ti=ti, nbk=nbk: e.tensor_tensor(
                            out=tmp[ti][:], in0=py[yb][:], in1=gtbc[:, gi, nbk * 512:(nbk + 1) * 512], op=ALU.mult),
                            reads=[R_py[yb], R_gtbc], writes=[R_tmp[ti]])
                        S.op("pool", lambda e, ti=ti, nbk=nbk, b=b: e.tensor_tensor(
                            out=xt[xb3][:, nbk * 512:(nbk + 1) * 512], in0=xt[xb3][:, nbk * 512:(nbk + 1) * 512], in1=tmp[ti][:], op=ALU.add),
                            reads=[R_tmp[ti], R_xt[xb3]], writes=[R_xt[xb3]])
                    S.dma("sp", lambda e, t=t, b=b: e.dma_start(out=resid[t * 128:(t + 1) * 128, :], in_=xt[xb3][:]),
                          reads=[R_xt[xb3]], writes=[R_res[t // 4]])

                a3_loads(0)
                a3_loads(1)
                a3_stage1(0)
                for t in range(32):
                    if t + 2 < 32:
                        a3_loads(t + 2)
                    if t + 1 < 32:
                        a3_stage1(t + 1)
                    a3_stage2(t)
                S.barrier()
                S.emit()

        def moe_phase(l, final):
            with ExitStack() as st:
                T, P = mkTP(st)

                wr_f = T("wr_f", [128, 8, 20], F32)
                wr_sb = T("wr_sb", [128, 8, 20], BF16)
                br_sb = T("br_sb", [128, 40], F32)
                xt = [T(f"xt{i}", [128, 4, DM], F32) for i in range(2)]
                xn = T("xn", [128, 4, DM], BF16)
                hT = [T(f"hT{i}", [128, 8, 512], BF16) for i in range(2)]
                junk = T("junk", [128, DM], BF16)
                ss = [T(f"ss{i}", [128, 4], F32) for i in range(2)]
                rstd = [T(f"rstd{i}", [128, 4], F32) for i in range(2)]
                actT = T("actT", [128, 32, 512], BF16)
                Gb = [T("Gb0", [128, 4, 16, 128], BF16)] * 2
                NWG = 3
                wgu = [T(f"wgu{i}", [128, 8, 512], BF16) for i in range(NWG)]
                NWD = 6
                wd = [T(f"wd{i}", [128, 2, 512], BF16) for i in range(NWD)]
                sg = [T(f"sg{i}", [128, 512], F32) for i in range(2)]
                tg_ = [T(f"tg{i}", [128, 512], F32) for i in range(2)]
                tmp = [T(f"tmp{i}", [128, 512], F32) for i in range(2)]
                rt = T("rt", [128, 4, 96], F32)
                gate = T("gate", [128, 4, 16], F32)
                pmain = [P(f"pm{i}", [128, 512], F32) for i in range(6)]
                pT = [P(f"pT{i}", [128, 1024], BF16) for i in range(1)]
                pR = P("pR", [128, 512], F32)
                R_wr = Reg("wr")
                R_xt = [Reg("xt0"), Reg("xt1")]
                R_xn = Reg("xn")
                R_hT = [Reg("hT0"), Reg("hT1")]
                R_junk = Reg("junk")
                R_ss = [Reg("ss0"), Reg("ss1")]
                R_act = [Reg(f"act{c}") for c in range(32)]
                R_Gb = [Reg("Gb0")] * 2
                R_wgu = [Reg(f"wgu{i}") for i in range(NWG)]
                R_wd = [Reg(f"wd{i}") for i in range(NWD)]
                R_sg = [Reg("sg0"), Reg("sg1")]
                R_tg = [Reg("tg0"), Reg("tg1")]
                R_tmp = [Reg("tmp0"), Reg("tmp1")]
                R_rt = Reg("rt")
                R_gate = Reg("gate")
                R_pm = [Reg(f"pm{i}") for i in range(6)]
                R_pT = [Reg("pT0")]
                R_pR = Reg("pR")

                S.dma("sp", lambda e: e.dma_start(out=wr_f[:], in_=wr[l].rearrange("(p k) n -> p k n", k=8)), writes=[R_wr])
                S.dma("sp", lambda e: e.dma_start(out=br_sb[:], in_=brr.partition_broadcast(128)), writes=[R_wr])
                S.op("dve", lambda e: e.tensor_copy(out=wr_sb[:], in_=wr_f[:]), reads=[R_wr], writes=[R_wr])
                a0, s0 = A_of(l, "f"), sh_of(l, "f")
                gi = gt_idx(l, "f")
                R_eg, R_eu, R_ed = R_w[f"eg{l}"], R_w[f"eu{l}"], R_w[f"ed{l}"]
                wgc = 0
                wdc = 0
                sgc = 0
                tmc = 0

                def load_x(t):
                    b = t % 2
                    S.dma("sp", lambda e: e.dma_start(
                        out=xt[b][:], in_=resid[t * 512:(t + 1) * 512, :].rearrange("(s p) d -> p s d", p=128)),
                        reads=[R_res[t]], writes=[R_xt[b]])

                gu_next = [0]
                d_next = [0]

                def issue_gu(upto):
                    while gu_next[0] < min(upto, 128):
                        i = gu_next[0]
                        gu_next[0] += 1
                        ex, wi = i % 16, i % NWG
                        S.dma("sp", lambda e: e.dma_start(
                            out=wgu[wi][:, :, 0:256], in_=eg_b[l, ex].rearrange("(p k) f -> p k f", k=8)),
                            reads=[R_eg], writes=[R_wgu[wi]])
                        S.dma("sp", lambda e: e.dma_start(
                            out=wgu[wi][:, :, 256:512], in_=eu_b[l, ex].rearrange("(p k) f -> p k f", k=8)),
                            reads=[R_eu], writes=[R_wgu[wi]])

                def issue_d(upto):
                    while d_next[0] < min(upto, 256):
                        i = d_next[0]
                        d_next[0] += 1
                        ex, nbk, di = i % 16, (i // 16) % 2, i % NWD
                        S.dma("sp", lambda e: e.dma_start(
                            out=wd[di][:], in_=ed_b[l, ex, :, nbk * 512:(nbk + 1) * 512].rearrange("(c p) d -> p c d", p=128)),
                            reads=[R_ed], writes=[R_wd[di]])

                def norm_router(t):
                    b = t % 2
                    norm_transpose(xt[b], R_xt[b], xn, R_xn, hT[b], R_hT[b], [pT[0], pT[0]], [R_pT[0], R_pT[0]],
                                   junk, R_junk, ss[b], rstd[b], R_ss[b],
                                   modv2[:, l * 16 + 8:l * 16 + 16], modv2[:, l * 16:l * 16 + 8], perm=True)
                    for s in range(4):
                        for k in range(8):
                            S.op("pe", lambda e, s=s, k=k, b=b: e.matmul(
                                pR[:, s * 32:s * 32 + 20], lhsT=hT[b][:, k, s * 128:(s + 1) * 128], rhs=wr_sb[:, k, :],
                                start=(k == 0), stop=(k == 7)),
                                reads=[R_hT[b], R_wr], writes=[R_pR] if k == 0 else [], sig=(k == 7))
                    lg = rt[:, :, 0:20]
                    S.op("dve", lambda e: e.tensor_tensor(
                        out=lg, in0=pR[:, 0:128].rearrange("p (s c) -> p s c", c=32)[:, :, 0:20],
                        in1=br_sb[:, l * 20:(l + 1) * 20].unsqueeze(1).to_broadcast([128, 4, 20]), op=ALU.add),
                        reads=[R_pR, R_wr], writes=[R_rt])
                    gl = rt[:, :, 0:4]
                    el = rt[:, :, 4:20]
                    gmx = rt[:, :, 20:21]
                    S.op("dve", lambda e: e.tensor_reduce(out=rt[:, :, 20:21], in_=gl, axis=AX.X, op=ALU.max), reads=[R_rt], writes=[R_rt])
                    ohg = rt[:, :, 24:28]
                    S.op("dve", lambda e: e.tensor_tensor(out=ohg, in0=gl, in1=gmx.to_broadcast([128, 4, 4]), op=ALU.is_equal),
                         reads=[R_rt], writes=[R_rt])
                    gsh = rt[:, :, 28:32]
                    S.op("dve", lambda e: e.tensor_tensor(out=gsh, in0=gl, in1=gmx.to_broadcast([128, 4, 4]), op=ALU.subtract),
                         reads=[R_rt], writes=[R_rt])
                    S.op("act", lambda e: e.activation(out=gsh, in_=gsh, func=AF.Exp), reads=[R_rt], writes=[R_rt])
                    S.op("dve", lambda e: e.tensor_reduce(out=rt[:, :, 21:22], in_=gsh, axis=AX.X, op=ALU.add), reads=[R_rt], writes=[R_rt])
                    S.op("dve", lambda e: e.reciprocal(out=rt[:, :, 21:22], in_=rt[:, :, 21:22]), reads=[R_rt], writes=[R_rt])
                    msk = rt[:, :, 32:48]
                    S.op("dve", lambda e: e.tensor_scalar(
                        out=msk.rearrange("p s (g x) -> p s g x", x=4), in0=ohg.unsqueeze(3).to_broadcast([128, 4, 4, 4]),
                        scalar1=-1.0, scalar2=BIG, op0=ALU.add, op1=ALU.mult), reads=[R_rt], writes=[R_rt])
                    elm = rt[:, :, 48:64]
                    S.op("dve", lambda e: e.tensor_tensor(out=elm, in0=el, in1=msk, op=ALU.add), reads=[R_rt], writes=[R_rt])
                    S.op("dve", lambda e: e.tensor_reduce(out=rt[:, :, 22:23], in_=elm, axis=AX.X, op=ALU.max), reads=[R_rt], writes=[R_rt])
                    oh1 = rt[:, :, 64:80]
                    S.op("dve", lambda e: e.tensor_tensor(out=oh1, in0=elm, in1=rt[:, :, 22:23].to_broadcast([128, 4, 16]), op=ALU.is_equal),
                         reads=[R_rt], writes=[R_rt])
                    elm2 = rt[:, :, 32:48]
                    S.op("dve", lambda e: e.scalar_tensor_tensor(out=elm2, in0=oh1, scalar=-BIG, in1=elm, op0=ALU.mult, op1=ALU.add),
                         reads=[R_rt], writes=[R_rt])
                    S.op("dve", lambda e: e.tensor_reduce(out=rt[:, :, 23:24], in_=elm2, axis=AX.X, op=ALU.max), reads=[R_rt], writes=[R_rt])
                    oh2 = rt[:, :, 80:96]
                    S.op("dve", lambda e: e.tensor_tensor(out=oh2, in0=elm2, in1=rt[:, :, 23:24].to_broadcast([128, 4, 16]), op=ALU.is_equal),
                         reads=[R_rt], writes=[R_rt])
                    dd = rt[:, :, 28:29]
                    S.op("dve", lambda e: e.tensor_tensor(out=dd, in0=rt[:, :, 23:24], in1=rt[:, :, 22:23], op=ALU.subtract),
                         reads=[R_rt], writes=[R_rt])
                    S.op("act", lambda e: e.activation(out=dd, in_=dd, func=AF.Exp), reads=[R_rt], writes=[R_rt])
                    p1 = rt[:, :, 29:30]
                    S.op("dve", lambda e: e.tensor_scalar(out=p1, in0=dd, scalar1=1.0, scalar2=None, op0=ALU.add), reads=[R_rt], writes=[R_rt])
                    S.op("dve", lambda e: e.reciprocal(out=p1, in_=p1), reads=[R_rt], writes=[R_rt])
                    p2 = rt[:, :, 30:31]
                    S.op("dve", lambda e: e.tensor_tensor(out=p2, in0=dd, in1=p1, op=ALU.mult), reads=[R_rt], writes=[R_rt])
                    S.op("dve", lambda e: e.tensor_tensor(out=p1, in0=p1, in1=rt[:, :, 21:22], op=ALU.mult), reads=[R_rt], writes=[R_rt])
                    S.op("dve", lambda e: e.tensor_tensor(out=p2, in0=p2, in1=rt[:, :, 21:22], op=ALU.mult), reads=[R_rt], writes=[R_rt])
                    S.op("dve", lambda e: e.tensor_tensor(out=oh1, in0=oh1, in1=p1.to_broadcast([128, 4, 16]), op=ALU.mult),
                         reads=[R_rt], writes=[R_rt])
                    S.op("dve", lambda e: e.tensor_tensor(out=oh2, in0=oh2, in1=p2.to_broadcast([128, 4, 16]), op=ALU.mult),
                         reads=[R_rt], writes=[R_rt])
                    S.op("dve", lambda e: e.tensor_tensor(out=gate[:], in0=oh1, in1=oh2, op=ALU.add), reads=[R_rt], writes=[R_gate])
                    S.op("pool", lambda e, b=b: e.tensor_copy(out=Gb[b][:], in_=gate[:].unsqueeze(3).to_broadcast([128, 4, 16, 128])),
                         reads=[R_gate], writes=[R_Gb[b]])

                def experts(t, hook):
                    b = t % 2
                    for ex in range(16):
                        gi_ = t * 16 + ex
                        wi = gi_ % NWG
                        issue_gu(gi_ + 1)
                        pgb = 4 + ex % 2
                        for s in range(4):
                            S.op("pe", lambda e, s=s, ex=ex, pgb=pgb, b=b: e.matmul(
                                pmain[pgb][:, s * 128:(s + 1) * 128], lhsT=Gb[b][:, s, ex, :], rhs=ident[:], start=True, stop=True),
                                reads=[R_Gb[b], R_const], writes=[R_pm[pgb]] if s == 0 else [], sig=(s == 3))
                        for fc in range(2):
                            c = ex * 2 + fc
                            hb = c % 2
                            for which, pbk in (("g", hb), ("u", 2 + hb)):
                                off = fc * 128 + (256 if which == "u" else 0)
                                for k in range(8):
                                    S.op("pe", lambda e, k=k, off=off, pbk=pbk, wi=wi, b=b: e.matmul(
                                        pmain[pbk][:], lhsT=wgu[wi][:, k, off:off + 128], rhs=hT[b][:, k, :],
                                        start=(k == 0), stop=(k == 7)),
                                        reads=[R_wgu[wi], R_hT[b]], writes=[R_pm[pbk]] if k == 0 else [], sig=(k == 7))
                            si = sgc_[0] % 2
                            sgc_[0] += 1
                            S.op("act", lambda e, hb=hb, si=si: e.activation(out=sg[si][:], in_=pmain[hb][:], func=AF.Silu),
                                 reads=[R_pm[hb]], writes=[R_sg[si]])
                            S.op("dve", lambda e, si=si, pgb=pgb: e.tensor_tensor(out=tg_[si][:], in0=sg[si][:], in1=pmain[pgb][:], op=ALU.mult),
                                 reads=[R_sg[si], R_pm[pgb]], writes=[R_tg[si]])
                            S.op("dve", lambda e, si=si, hb=hb, c=c: e.tensor_tensor(out=actT[:, c, :], in0=tg_[si][:], in1=pmain[2 + hb][:], op=ALU.mult),
                                 reads=[R_tg[si], R_pm[2 + hb]], writes=[R_act[c]])
                        issue_gu(gi_ + NWG + 1)
                        if ex == 2:
                            hook()

                def down(t):
                    b = t % 2
                    for nbk in range(2):
                        for ex in range(16):
                            di_ = t * 32 + nbk * 16 + ex
                            di = di_ % NWD
                            issue_d(di_ + 1)
                            for fc in range(2):
                                c = ex * 2 + fc
                                for s in range(4):
                                    S.op("pe", lambda e, c=c, s=s, di=di, fc=fc: e.matmul(
                                        pmain[s][:], lhsT=actT[:, c, s * 128:(s + 1) * 128], rhs=wd[di][:, fc, :],
                                        start=(c == 0), stop=(c == 31)),
                                        reads=[R_act[c], R_wd[di]], writes=[R_pm[s]] if c in (0, 31) else [], sig=(c == 31 or s == 3))
                            issue_d(di_ + NWD + 1)
                        for s in range(4):
                            ti = tmc_[0] % 2
                            tmc_[0] += 1
                            S.op("dve", lambda e, s=s, ti=ti, nbk=nbk: e.tensor_tensor(
                                out=tmp[ti][:], in0=pmain[s][:], in1=gtbc[:, gi, nbk * 512:(nbk + 1) * 512], op=ALU.mult),
                                reads=[R_pm[s], R_gtbc], writes=[R_tmp[ti]])
                            S.op("pool", lambda e, s=s, ti=ti, nbk=nbk, b=b: e.tensor_tensor(
                                out=xt[b][:, s, nbk * 512:(nbk + 1) * 512], in0=xt[b][:, s, nbk * 512:(nbk + 1) * 512],
                                in1=tmp[ti][:], op=ALU.add), reads=[R_tmp[ti], R_xt[b]], writes=[R_xt[b]])

                def finish(t):
                    b = t % 2
                    if not final:
                        return lambda: S.dma("sp", lambda e: e.dma_start(
                            out=resid[t * 512:(t + 1) * 512, :].rearrange("(s p) d -> p s d", p=128), in_=xt[b][:]),
                            reads=[R_xt[b]], writes=[R_res[t]])
                    else:
                        for s in range(4):
                            S.op("act", lambda e, s=s, b=b: e.activation(out=junk[:], in_=xt[b][:, s, :], func=AF.Square,
                                                                          accum_out=ss[b][:, s:s + 1]),
                                 reads=[R_xt[b]], writes=[R_junk, R_ss[b]])
                        S.op("act", lambda e, b=b: e.activation(out=rstd[b][:], in_=ss[b][:], func=AF.Sqrt, scale=1.0 / DM, bias=EPS),
                             reads=[R_ss[b]], writes=[R_ss[b]])
                        S.op("dve", lambda e, b=b: e.reciprocal(out=rstd[b][:], in_=rstd[b][:]), reads=[R_ss[b]], writes=[R_ss[b]])
                        for s in range(4):
                            S.op("dve", lambda e, s=s, b=b: e.scalar_tensor_tensor(
                                out=xt[b][:, s, :], in0=xt[b][:, s, :], scalar=rstd[b][:, s:s + 1], in1=finbc[:],
                                op0=ALU.mult, op1=ALU.mult), reads=[R_xt[b], R_ss[b], R_const], writes=[R_xt[b]])
                        return lambda: S.dma("sp", lambda e: e.dma_start(
                            out=out_d[t * 512:(t + 1) * 512, :].rearrange("(s p) d -> p s d", p=128), in_=xt[b][:]),
                            reads=[R_xt[b]], writes=[R_out])

                sgc_ = [0]
                tmc_ = [0]
                load_x(0)
                issue_gu(NWG)
                issue_d(NWD)
                norm_router(0)
                pend = []
                for t in range(8):
                    def hook(t=t):
                        for st_ in pend:
                            st_()
                        pend.clear()
                        if t + 1 < 8:
                            load_x(t + 1)
                    experts(t, hook)
                    if t + 1 < 8:
                        norm_router(t + 1)
                    down(t)
                    pend.append(finish(t))
                for st_ in pend:
                    st_()
                S.barrier()
                S.emit()

        if nphase >= 2:
            moe_phase(0, final=(nphase == 2))

        if nphase >= 3:
            with ExitStack() as st:
                T, P = mkTP(st)

                win_sb = T("win_sb", [128, 8, 4096], BF16)
                bwoc = [T(f"bwoc{i}", [128, 4, 512], BF16) for i in range(3)]
                junk2 = T("junk2", [128, DM], BF16)
                ws_f = T("ws_f", [128, 8, 128], F32)
                ws_b = T("ws_b", [128, 8, 128], BF16)
                wsT = T("wsT", [128, 8, 128], BF16)
                bs_f = T("bs_f", [1, 1024], F32)
                bs_b = T("bs_b", [1, 1024], BF16)
                ones = T("ones", [1, 128], BF16)
                gain_f = T("gain_f", [128, 512], F32)
                gain = T("gain", [128, 2048], BF16)
                xt = [T(f"xt{i}", [128, 4, DM], F32) for i in range(2)]
                hT = T("hT", [128, 8, 512], BF16)
                ss = T("ss", [128, 4], F32)
                rstd = T("rstd", [128, 4], F32)
                uT = T("uT", [128, 16, 512], BF16)
                vf0 = T("vf0", [128, 2048], F32)
                vf = [vf0, vf0]
                xn = vf0[:].bitcast(BF16).rearrange("p (s d) -> p s d", d=DM)
                vn = T("vn", [128, 4, 2048], BF16)
                junk = vn[:, 3, :]
                st_ = T("st_", [128, 16], F32)
                tmp = [T(f"tmp{i}", [128, 512], F32) for i in range(2)]
                pT = [P(f"pT{i}", [128, 1024], BF16) for i in range(2)]
                pm = [P(f"pm{i}", [128, 512], F32) for i in range(6)]
                R_win, R_ws, R_gain = Reg("win"), Reg("ws"), Reg("gain")
                R_bwoc = [Reg(f"bwoc{i}") for i in range(3)]
                R_junk2 = Reg("junk2")
                R_xt = [Reg("xt0"), Reg("xt1")]
                R_hT, R_ss = Reg("hT"), Reg("ss")
                R_uT = [Reg(f"uT{c}") for c in range(16)]
                R_vf = [Reg("vf0")] * 2
                R_xn = R_vf[0]
                R_vn = [Reg(f"vn{s}") for s in range(4)]
                R_junk = R_vn[3]
                R_st = Reg("st")
                R_tmp = [Reg("tmp0"), Reg("tmp1")]
                R_pT = [Reg("pT0"), Reg("pT1")]
                R_pm = [Reg(f"pm{i}") for i in range(6)]

                for k in range(8):
                    S.dma("sp", lambda e, k=k: e.dma_start(out=win_sb[:, k, :], in_=win_b[k * 128:(k + 1) * 128, :]),
                          reads=[R_w["win_b"]], writes=[R_win])
                S.dma("sp", lambda e: e.dma_start(out=ws_f[:], in_=ws.rearrange("g p q -> p g q")), writes=[R_ws])
                S.dma("sp", lambda e: e.dma_start(out=bs_f[:], in_=bsr), writes=[R_ws])
                S.dma("sp", lambda e: e.dma_start(out=ones[:], in_=ones_d), writes=[R_ws])
                for q4 in range(4):
                    S.dma("sp", lambda e, q4=q4: e.dma_start(out=gain_f[:], in_=vgain[:, q4 * 512:(q4 + 1) * 512].partition_broadcast(128)),
                          writes=[R_gain])
                    S.op("dve", lambda e, q4=q4: e.tensor_copy(out=gain[:, q4 * 512:(q4 + 1) * 512], in_=gain_f[:]), reads=[R_gain], writes=[R_gain])
                S.op("dve", lambda e: e.tensor_copy(out=ws_b[:], in_=ws_f[:]), reads=[R_ws], writes=[R_ws])
                S.op("dve", lambda e: e.tensor_copy(out=bs_b[:], in_=bs_f[:]), reads=[R_ws], writes=[R_ws])
                for g in range(8):
                    S.op("pe", lambda e, g=g: e.transpose(pT[0][:, g * 128:(g + 1) * 128], ws_b[:, g, :], ident[:]),
                         reads=[R_ws, R_const], writes=[R_pT[0]] if g == 0 else [], sig=(g == 7))
                S.op("act", lambda e: e.copy(out=wsT[:], in_=pT[0][:].rearrange("p (g q) -> p g q", q=128)),
                     reads=[R_pT[0]], writes=[R_ws])
                a0, s0 = A_of(1, "m"), sh_of(1, "m")
                gi = gt_idx(1, "m")
                pmc_ = [0]
                tmc_ = [0]
                bw_next = [0]
                NBW = 3

                def issue_bw(upto):
                    while bw_next[0] < min(upto, 64):
                        i = bw_next[0]
                        bw_next[0] += 1
                        j, nbk, di = i % 4, (i // 4) % 2, i % NBW
                        S.dma("sp", lambda e: e.dma_start(
                            out=bwoc[di][:], in_=bwo_b[j * 512:(j + 1) * 512, nbk * 512:(nbk + 1) * 512].rearrange("(c p) n -> p c n", p=128)),
                            reads=[R_w["bwo_b"]], writes=[R_bwoc[di]])

                def g_load(t):
                    b = t % 2
                    S.dma("sp", lambda e: e.dma_start(
                        out=xt[b][:], in_=resid[t * 512:(t + 1) * 512, :].rearrange("(s p) d -> p s d", p=128)),
                        reads=[R_res[t]], writes=[R_xt[b]])

                def g_norm(t):
                    b = t % 2
                    norm_transpose(xt[b], R_xt[b], xn, R_xn, hT, R_hT, pT, R_pT, junk2, R_junk2, ss, rstd, R_ss, Av[:, 8:16], modv[:, 48:56])

                def g_v(t):
                    for s in range(4):
                        vb = s % 2
                        for nbk in range(4):
                            pb = pmc_[0] % 6
                            pmc_[0] += 1
                            for k in range(8):
                                S.op("pe", lambda e, s=s, k=k, nbk=nbk, pb=pb: e.matmul(
                                    pm[pb][:], lhsT=hT[:, k, s * 128:(s + 1) * 128],
                                    rhs=win_sb[:, k, 2048 + nbk * 512:2048 + (nbk + 1) * 512], start=(k == 0), stop=(k == 7)),
                                    reads=[R_hT, R_win], writes=[R_pm[pb]] if k == 0 else [], sig=(k == 7))
                            S.op("act", lambda e, nbk=nbk, pb=pb, vb=vb: e.activation(
                                out=vf[vb][:, nbk * 512:(nbk + 1) * 512], in_=pm[pb][:], func=AF.Gelu,
                                accum_out=st_[:, nbk:nbk + 1]), reads=[R_pm[pb]], writes=[R_vf[vb], R_st])
                        S.op("act", lambda e, vb=vb: e.activation(out=vn[:, s, :], in_=vf[vb][:], func=AF.Square, accum_out=st_[:, 4:5]),
                             reads=[R_vf[vb]], writes=[R_vn[s], R_st])
                        S.op("dve", lambda e: e.tensor_reduce(out=st_[:, 5:6], in_=st_[:, 0:4], axis=AX.X, op=ALU.add), reads=[R_st], writes=[R_st])
                        S.op("dve", lambda e: e.tensor_scalar(out=st_[:, 5:6], in0=st_[:, 5:6], scalar1=1.0 / 2048, scalar2=None, op0=ALU.mult),
                             reads=[R_st], writes=[R_st])
                        S.op("dve", lambda e: e.tensor_tensor(out=st_[:, 6:7], in0=st_[:, 5:6], in1=st_[:, 5:6], op=ALU.mult), reads=[R_st], writes=[R_st])
                        S.op("dve", lambda e: e.scalar_tensor_tensor(out=st_[:, 7:8], in0=st_[:, 4:5], scalar=1.0 / 2048, in1=st_[:, 6:7],
                                                                      op0=ALU.mult, op1=ALU.subtract), reads=[R_st], writes=[R_st])
                        S.op("act", lambda e: e.activation(out=st_[:, 8:9], in_=st_[:, 7:8], func=AF.Sqrt, bias=EPS), reads=[R_st], writes=[R_st])
                        S.op("dve", lambda e: e.reciprocal(out=st_[:, 8:9], in_=st_[:, 8:9]), reads=[R_st], writes=[R_st])
                        S.op("dve", lambda e, vb=vb: e.tensor_scalar(out=vf[vb][:], in0=vf[vb][:], scalar1=st_[:, 5:6], scalar2=st_[:, 8:9],
                                                                      op0=ALU.subtract, op1=ALU.mult), reads=[R_st, R_vf[vb]], writes=[R_vf[vb]])
                        S.op("pool", lambda e, vb=vb, s=s: e.tensor_tensor(out=vn[:, s, :], in0=vf[vb][:], in1=gain[:], op=ALU.mult),
                             reads=[R_vf[vb], R_gain], writes=[R_vn[s]])

                def g_u(t):
                    for mt in range(16):
                        pb = pmc_[0] % 6
                        pmc_[0] += 1
                        for k in range(8):
                            S.op("pe", lambda e, mt=mt, k=k, pb=pb: e.matmul(
                                pm[pb][:], lhsT=win_sb[:, k, mt * 128:(mt + 1) * 128], rhs=hT[:, k, :], start=(k == 0), stop=(k == 7)),
                                reads=[R_win, R_hT], writes=[R_pm[pb]] if k == 0 else [], sig=(k == 7))
                        S.op("act", lambda e, mt=mt, pb=pb: e.activation(out=uT[:, mt, :], in_=pm[pb][:], func=AF.Gelu),
                             reads=[R_pm[pb]], writes=[R_uT[mt]])

                def g_spatial(t):
                    for cc in range(16):
                        g = cc // 2
                        pb = pmc_[0] % 6
                        pmc_[0] += 1
                        for s in range(4):
                            S.op("pe", lambda e, cc=cc, s=s, g=g, pb=pb: e.matmul(
                                pm[pb][:, s * 128:(s + 1) * 128], lhsT=vn[:, s, cc * 128:(cc + 1) * 128], rhs=wsT[:, g, :],
                                start=True, stop=False), reads=[R_vn[s], R_ws], writes=[R_pm[pb]] if s == 0 else [], sig=False)
                            S.op("pe", lambda e, s=s, g=g, pb=pb: e.matmul(
                                pm[pb][:, s * 128:(s + 1) * 128], lhsT=ones[:], rhs=bs_b[:, g * 128:(g + 1) * 128],
                                start=False, stop=True), reads=[R_ws], writes=[], sig=(s == 3))
                        S.op("dve", lambda e, cc=cc, pb=pb: e.tensor_tensor(out=uT[:, cc, :], in0=uT[:, cc, :], in1=pm[pb][:], op=ALU.mult),
                             reads=[R_pm[pb], R_uT[cc]], writes=[R_uT[cc]])

                def g_out(t):
                    b = t % 2
                    for nbk in range(2):
                        for j in range(4):
                            i = t * 8 + nbk * 4 + j
                            di = i % NBW
                            issue_bw(i + 1)
                            for c4 in range(4):
                                cc = j * 4 + c4
                                for s in range(4):
                                    S.op("pe", lambda e: e.matmul(
                                        pm[s][:], lhsT=uT[:, cc, s * 128:(s + 1) * 128], rhs=bwoc[di][:, c4, :],
                                        start=(cc == 0), stop=(cc == 15)),
                                        reads=[R_uT[cc], R_bwoc[di]], writes=[R_pm[s]] if cc in (0, 15) else [],
                                        sig=(cc == 15 or (c4 == 3 and s == 3)))
                            issue_bw(i + NBW + 1)
                        for s in range(4):
                            ti = tmc_[0] % 2
                            tmc_[0] += 1
                            S.op("dve", lambda e: e.tensor_tensor(
                                out=tmp[ti][:], in0=pm[s][:], in1=gtbc[:, gi, nbk * 512:(nbk + 1) * 512], op=ALU.mult),
                                reads=[R_pm[s], R_gtbc], writes=[R_tmp[ti]])
                            S.op("pool", lambda e: e.tensor_tensor(
                                out=xt[b][:, s, nbk * 512:(nbk + 1) * 512], in0=xt[b][:, s, nbk * 512:(nbk + 1) * 512], in1=tmp[ti][:], op=ALU.add),
                                reads=[R_tmp[ti], R_xt[b]], writes=[R_xt[b]])
                    S.dma("sp", lambda e: e.dma_start(
                        out=resid[t * 512:(t + 1) * 512, :].rearrange("(s p) d -> p s d", p=128), in_=xt[b][:]),
                        reads=[R_xt[b]], writes=[R_res[t]])

                g_load(0)
                issue_bw(NBW)
                g_norm(0)
                for t in range(8):
                    if t + 1 < 8:
                        g_load(t + 1)
                    g_v(t)
                    g_u(t)
                    if t + 1 < 8:
                        g_norm(t + 1)
                    g_spatial(t)
                    g_out(t)
                S.barrier()
                S.emit()

        if nphase >= 4:
            moe_phase(1, final=True)

        S.barrier(final=True)
        S.emit()
    return nc


_CACHE = {}


def _consts():
    bf = ml_dtypes.bfloat16
    ident = np.eye(128, dtype=np.float32).astype(bf)
    kp = np.arange(128)[:, None]
    tq = np.arange(256)[None, :]
    band = (((tq - kp) >= 0) & ((tq - kp) <= 128)).astype(np.float32).astype(bf)
    ones = np.ones((1, 128), np.float32).astype(bf)
    return ident, band, ones


def _prep_inputs(inp):
    f32 = np.float32
    x = np.asarray(inp["x"], f32)
    c = np.asarray(inp["c"], f32)
    ident, band, ones = _consts()
    inv_freq = (np.float32(500000.0) ** (-(np.arange(0, 16, 2, dtype=np.float32)) / np.float32(16))).astype(f32)
    b_ada = np.asarray(inp["b_ada"], f32)
    b_ada_l = np.ascontiguousarray(b_ada.reshape(2, 48, 128).transpose(2, 0, 1).reshape(128, 96))
    nrm = np.concatenate([np.asarray(inp["norm_mix"], f32), np.asarray(inp["norm_ffn"], f32),
                          np.asarray(inp["final_norm"], f32)[None]], 0)
    nrm_l = np.ascontiguousarray(nrm.reshape(5, 8, 128).transpose(2, 0, 1).reshape(128, 40))
    wr = np.concatenate([np.asarray(inp["r_w_group"], f32),
                         np.asarray(inp["r_w_expert"], f32).transpose(0, 2, 1, 3).reshape(2, DM, 16)], -1)
    brr = np.concatenate([np.asarray(inp["r_b_group"], f32), np.asarray(inp["r_b_expert"], f32).reshape(2, 16)], -1).reshape(1, 40)
    shared = dict(
        w_ada=np.ascontiguousarray(inp["w_ada"], f32), b_ada_l=b_ada_l, b_ada_r=np.ascontiguousarray(b_ada),
        nrm=nrm_l, fin_row=np.asarray(inp["final_norm"], f32).reshape(1, DM),
        wqkv=np.ascontiguousarray(inp["a_w_qkv"][0], f32), wo=np.ascontiguousarray(inp["a_w_o"][0], f32),
        win=np.ascontiguousarray(inp["b_w_in"][0], f32), vgain=np.asarray(inp["b_v_gain"], f32).reshape(1, 2048),
        ws=np.ascontiguousarray(inp["b_w_s"][0], f32), bsr=np.asarray(inp["b_b_s"][0], f32).reshape(1, 1024),
        bwo=np.ascontiguousarray(inp["b_w_o"][0], f32), wr=np.ascontiguousarray(wr), brr=np.ascontiguousarray(brr),
        eg=np.ascontiguousarray(inp["e_w_gate"], f32), eu=np.ascontiguousarray(inp["e_w_up"], f32),
        ed=np.ascontiguousarray(inp["e_w_down"], f32), ident=ident, band=band, ones=ones,
        maskA=(np.arange(DM)[None, :] % 128 == np.arange(128)[:, None]).astype(f32),
        maskB=(np.arange(DM)[None, :] // 8 == np.arange(128)[:, None]).astype(f32),
        nrm2=np.ascontiguousarray(np.asarray(inp["norm_ffn"], f32).reshape(2, 128, 8).transpose(1, 0, 2).reshape(128, 16)),
    )
    maps = []
    for ci in range(NCORES):
        b, T0 = ci // 4, (ci % 4) * TOWN
        pos = T0 - HALO + np.arange(TEXT)
        valid = (pos >= 0) & (pos < SEQ)
        xe = np.zeros((TEXT, DM), f32)
        xe[valid] = x[b, pos[valid]]
        ang = pos.astype(f32)[:, None] * inv_freq[None, :]
        cs = np.concatenate([np.cos(ang), np.sin(ang)], -1).astype(f32)
        kval = np.ascontiguousarray(valid.astype(f32).reshape(48, 128).T)
        m = dict(shared)
        m.update(xext=xe, cvec=np.ascontiguousarray(c[b].reshape(8, 128).T), cs=cs, kval=kval)
        maps.append(m)
    return maps


def kernel(**inputs):
    if "nc" not in _CACHE:
        _CACHE["nc"] = build_program()
    nc = _CACHE["nc"]
    maps = _prep_inputs(inputs)
    res = run_bass_kernel_spmd(nc, maps, core_ids=list(range(NCORES)))
    out = np.empty((2, SEQ, DM), np.float32)
    for ci in range(NCORES):
        b, T0 = ci // 4, (ci % 4) * TOWN
        out[b, T0:T0 + TOWN] = res.results[ci]["out"]
    return out
```

```python
from contextlib import ExitStack

import numpy as np
import ml_dtypes

import concourse.bass as bass
import concourse.mybir as mybir
from concourse.bass_utils import run_bass_kernel_spmd

F32 = mybir.dt.float32
BF16 = mybir.dt.bfloat16
AF = mybir.ActivationFunctionType
ALU = mybir.AluOpType
AX = mybir.AxisListType

ENGS = ("pe", "act", "dve", "pool", "sp")
EPOCH = 30000
NDSEM = 12

SEQ = 16384
DM = 1024
TOWN = 4096
HALO = 1024
TEXT = TOWN + 2 * HALO
NCORES = 8
EPS = 1e-6
BIG = 1.0e4
SCRATCH_EXTERNAL = True


import types


def _freeze(fn):
    if fn is None or fn.__closure__ is None:
        return fn
    cells = []
    for c in fn.__closure__:
        try:
            cells.append(types.CellType(c.cell_contents))
        except ValueError:
            cells.append(c)
    return types.FunctionType(fn.__code__, fn.__globals__, fn.__name__, fn.__defaults__, tuple(cells))


class Reg:
    __slots__ = ("name", "w", "r", "excl")

    def __init__(self, name=""):
        self.name = name
        self.w = {}
        self.r = {}
        self.excl = name.startswith("p")


class Sched:
    def __init__(self, nc, stack):
        self.nc = nc
        self.stack = stack
        self.q = {e: [] for e in ENGS}
        self.cnt = {e: 0 for e in ENGS}
        self.epoch = {e: 0 for e in ENGS}
        self.sem = {}
        self.seen = {e: {} for e in ENGS}
        self.dsem = {}
        self.dcnt = {}
        self.bg = set()
        self.drr = {e: 0 for e in ENGS}
        for e in ENGS:
            self._newsem(e)

    def _newsem(self, e):
        k = (e, self.epoch[e])
        self.sem[k] = self.stack.enter_context(self.nc.semaphore(f"s_{e}_{self.epoch[e]}"))
        self.cnt[e] = 0

    def _handle(self, key):
        return self.dsem[key] if key[0] == "d" else self.sem[key]

    def _waits(self, e, deps):
        need = {}
        for t in deps:
            key, val = t
            if e == "pe" and key[0] == "pe":
                continue
            if self.seen[e].get(key, 0) >= val:
                continue
            if need.get(key, 0) < val:
                need[key] = val
        out = []
        for key, val in need.items():
            self.seen[e][key] = val
            out.append((self._handle(key), val))
        return out

    def _deps(self, reads, writes, extra, e=None):
        deps = list(extra)
        for r in reads:
            deps.extend(r.w.items())
            if r.excl:
                deps.extend((k, v) for k, v in r.r.items() if k[0] != e)
        for w in writes:
            deps.extend(w.w.items())
            deps.extend(w.r.items())
        return deps

    def _mark(self, tick, reads, writes):
        key, val = tick
        for r in reads:
            if r.r.get(key, 0) < val:
                r.r[key] = val
        for w in writes:
            w.w = {key: val}
            w.r = {}

    def op(self, e, fn, reads=(), writes=(), sig=True, extra=()):
        waits = self._waits(e, self._deps(reads, writes, extra, e))
        if sig:
            if self.cnt[e] >= EPOCH:
                self.epoch[e] += 1
                self._newsem(e)
            self.cnt[e] += 1
            key = (e, self.epoch[e])
            tick = (key, self.cnt[e])
        else:
            key = (e, self.epoch[e])
            tick = (key, self.cnt[e] + 1)
        self.q[e].append((waits, _freeze(fn), (self.sem[key], 1) if sig else None))
        self._mark(tick, reads, writes)
        return tick

    def dma(self, e, fn, reads=(), writes=(), extra=(), bg=False):
        waits = self._waits(e, self._deps(reads, writes, extra, e))
        anchor = writes[0] if writes else reads[0]
        key = ("d", id(anchor))
        if key not in self.dsem:
            self.dsem[key] = self.stack.enter_context(self.nc.semaphore(f"dq{len(self.dsem)}"))
            self.dcnt[key] = 0
        if bg:
            self.bg.add(key)
        self.dcnt[key] += 16
        tick = (key, self.dcnt[key])
        self.q[e].append((waits, _freeze(fn), (self.dsem[key], 16)))
        self._mark(tick, reads, writes)
        return tick

    def barrier(self, final=False):
        deps = []
        for e in ENGS:
            if self.cnt[e] > 0:
                deps.append(((e, self.epoch[e]), self.cnt[e]))
        for k, v in self.dcnt.items():
            if v > 0 and (final or k not in self.bg):
                deps.append((k, v))
        for e in ENGS:
            waits = self._waits(e, deps)
            if waits:
                self.q[e].append((waits, None, None))

    def emit(self):
        nc = self.nc
        engmap = {"pe": "tensor", "act": "scalar", "dve": "vector", "pool": "gpsimd", "sp": "sync"}
        with nc.Block() as block:
            for e in ENGS:
                items = self.q[e]
                if not items:
                    continue

                def body(eng, items=items):
                    for waits, fn, sig in items:
                        for h, v in waits:
                            eng.wait_ge(h, v)
                        if fn is None:
                            continue
                        ins = fn(eng)
                        if sig is not None:
                            ins.then_inc(sig[0], sig[1])

                getattr(block, engmap[e])(body)
        self.q = {e: [] for e in ENGS}


def build_program(nphase=5, dbg=False, asub=3):
    nc = bass.Bass("TRN2", target_bir_lowering=False)

    def din(name, shape, dt=F32):
        return nc.dram_tensor(name, list(shape), dt, kind="ExternalInput").ap()

    def dscr(name, shape, dt):
        return nc.dram_tensor(name, list(shape), dt, kind="Internal").ap()

    xext = din("xext", [TEXT, DM])
    cvec = din("cvec", [128, 8])
    w_ada = din("w_ada", [2, DM, 6 * DM])
    b_ada_l = din("b_ada_l", [128, 96])
    b_ada_r = din("b_ada_r", [2, 6 * DM])
    nrm = din("nrm", [128, 40])
    fin_row = din("fin_row", [1, DM])
    wqkv = din("wqkv", [DM, 2880])
    wo = din("wo", [960, DM])
    win = din("win", [DM, 4096])
    vgain = din("vgain", [1, 2048])
    ws = din("ws", [8, 128, 128])
    bsr = din("bsr", [1, 1024])
    bwo = din("bwo", [2048, DM])
    wr = din("wr", [2, DM, 20])
    brr = din("brr", [1, 40])
    eg = din("eg", [2, 16, DM, 256])
    eu = din("eu", [2, 16, DM, 256])
    ed = din("ed", [2, 16, 256, DM])
    cs = din("cs", [TEXT, 16])
    kval = din("kval", [128, 48])
    ident_d = din("ident", [128, 128], BF16)
    band_d = din("band", [128, 256], BF16)
    ones_d = din("ones", [1, 128], BF16)
    maskA_d = din("maskA", [128, DM])
    maskB_d = din("maskB", [128, DM])
    nrm2_d = din("nrm2", [128, 16])

    out_d = nc.dram_tensor("out", [TOWN, DM], F32, kind="ExternalOutput").ap()
    resid = nc.dram_tensor("resid", [TOWN, DM], F32, kind="ExternalOutput" if (dbg or SCRATCH_EXTERNAL) else "Internal").ap()

    wqkv_b = dscr("wqkv_b", [DM, 2880], BF16)
    wo_b = dscr("wo_b", [960, DM], BF16)
    win_b = dscr("win_b", [DM, 4096], BF16)
    bwo_b = dscr("bwo_b", [2048, DM], BF16)
    eg_b = dscr("eg_b", [2, 16, DM, 256], BF16)
    eu_b = dscr("eu_b", [2, 16, DM, 256], BF16)
    ed_b = dscr("ed_b", [2, 16, 256, DM], BF16)
    def dscr2(name, shape, dt):
        return nc.dram_tensor(name, list(shape), dt, kind="ExternalOutput" if (dbg or SCRATCH_EXTERNAL) else "Internal").ap()
    q_s = dscr2("q_s", [TOWN, 960], BF16)
    k_s = dscr2("k_s", [TEXT, 960], BF16)
    v_s = dscr2("v_s", [TEXT, 975], BF16)
    o_s = dscr2("o_s", [TOWN, 975], F32)

    with ExitStack() as gst:
        S = Sched(nc, gst)

        uid = [0]

        def mkTP(st):
            uid[0] += 1
            tag = f"ph{uid[0]}_"

            def T(name, shape, dt):
                return st.enter_context(nc.sbuf_tensor(tag + name, list(shape), dt))

            def P(name, shape, dt):
                return st.enter_context(nc.psum_tensor(tag + name, list(shape), dt))
            return T, P

        def GT(name, shape, dt):
            return gst.enter_context(nc.sbuf_tensor("g_" + name, list(shape), dt))

        ident = GT("ident_sb", [128, 128], BF16)
        modv = GT("modv", [128, 96], F32)
        Av = GT("Av", [128, 32], F32)
        modv2 = GT("modv2", [128, 32], F32)
        nrm_sb = GT("nrm_sb", [128, 40], F32)
        gtbc = GT("gtbc", [128, 4, DM], F32)
        finbc = GT("finbc", [128, DM], F32)
        R_const = Reg("const")
        R_mod = Reg("mod")
        R_gtbc = Reg("gtbc")
        R_w = {n: Reg(n) for n in ("wqkv_b", "wo_b", "win_b", "bwo_b", "eg0", "eu0", "ed0", "eg1", "eu1", "ed1")}
        R_q, R_k, R_v, R_o = Reg("q_s"), Reg("k_s"), Reg("v_s"), Reg("o_s")
        R_res = [Reg(f"res{t}") for t in range(8)]
        R_out = Reg("out")

        def sh_of(l, which):
            return l * 48 + (0 if which == "m" else 24)

        def gt_idx(l, which):
            return l * 2 + (0 if which == "m" else 1)

        def issue_big_casts():
            for l in range(2):
                for nm, src, dst in (("eg", eg, eg_b), ("eu", eu, eu_b), ("ed", ed, ed_b)):
                    S.dma("pool", lambda e: e.dma_start(
                        out=dst[l].rearrange("e a b -> (e a) b"), in_=src[l].rearrange("e a b -> (e a) b")),
                        writes=[R_w[f"{nm}{l}"]], bg=True)
                if l == 0:
                    S.dma("pool", lambda e: e.dma_start(out=win_b, in_=win), writes=[R_w["win_b"]], bg=True)
                    S.dma("pool", lambda e: e.dma_start(out=bwo_b, in_=bwo), writes=[R_w["bwo_b"]], bg=True)

        with ExitStack() as st:
            T, P = mkTP(st)

            S.dma("pool", lambda e: e.dma_start(out=wqkv_b, in_=wqkv), writes=[R_w["wqkv_b"]], bg=True)
            S.dma("pool", lambda e: e.dma_start(out=wo_b, in_=wo), writes=[R_w["wo_b"]], bg=True)
            S.dma("sp", lambda e: e.dma_start(out=ident[:], in_=ident_d), writes=[R_const])
            S.dma("sp", lambda e: e.dma_start(out=nrm_sb[:], in_=nrm), writes=[R_const])
            S.dma("sp", lambda e: e.dma_start(out=finbc[:], in_=fin_row.partition_broadcast(128)), writes=[R_const])

            cv = T("cv", [128, 8], F32)
            cact = T("cact", [128, 8], F32)
            cbc = T("cbc", [128, 8, 128], F32)
            maskA = T("maskA", [128, DM], F32)
            maskB = T("maskB", [128, DM], F32)
            nrm2 = T("nrm2", [128, 16], F32)
            modbc = T("modbc", [128, 2, 6 * DM], F32)
            NWA = 4
            bro = [T(f"bro{i}", [128, 512], F32) for i in range(NWA)]
            wa = [T(f"wa{i}", [128, 8, 512], F32) for i in range(NWA)]
            xtmp = [T(f"xtmp{i}", [128, DM], F32) for i in range(2)]
            sct = T("sct", [128, 8], F32)
            pbc = [P(f"pbc{i}", [128, 512], F32) for i in range(2)]
            R_cv, R_cact, R_cbc, R_msk, R_modbc, R_sct = (Reg(n) for n in ("cv", "cact", "cbc", "msk", "modbc", "sct"))
            R_wa = [Reg(f"wa{i}") for i in range(NWA)]
            R_bro = [Reg(f"bro{i}") for i in range(NWA)]
            R_xtmp = [Reg("xtmp0"), Reg("xtmp1")]
            R_pbc = [Reg("pbc0"), Reg("pbc1")]

            S.dma("sp", lambda e: e.dma_start(out=cv[:], in_=cvec), writes=[R_cv])
            S.dma("sp", lambda e: e.dma_start(out=maskA[:], in_=maskA_d), writes=[R_msk])
            S.dma("sp", lambda e: e.dma_start(out=maskB[:], in_=maskB_d), writes=[R_msk])
            S.dma("sp", lambda e: e.dma_start(out=nrm2[:], in_=nrm2_d), writes=[R_msk])
            S.op("act", lambda e: e.activation(out=cact[:], in_=cv[:], func=AF.Silu), reads=[R_cv], writes=[R_cact])
            S.op("dve", lambda e: e.tensor_copy(out=cbc[:], in_=cact[:].unsqueeze(2).to_broadcast([128, 8, 128])),
                 reads=[R_cact], writes=[R_cbc])
            blk_i = 0
            for l in range(2):
                for blk in range(12):
                    wb = blk_i % NWA
                    pb = blk_i % 2
                    blk_i += 1
                    S.dma("sp", lambda e: e.dma_start(
                        out=wa[wb][:], in_=w_ada[l, :, blk * 512:(blk + 1) * 512].rearrange("(k p) n -> p k n", p=128)),
                        writes=[R_wa[wb]])
                    S.dma("sp", lambda e: e.dma_start(
                        out=bro[wb][:], in_=b_ada_r[l:l + 1, blk * 512:(blk + 1) * 512].partition_broadcast(128)),
                        writes=[R_bro[wb]])
                    for k in range(8):
                        S.op("pe", lambda e: e.matmul(
                            pbc[pb][:], lhsT=cbc[:, k, :], rhs=wa[wb][:, k, :], start=(k == 0), stop=(k == 7)),
                            reads=[R_wa[wb], R_cbc], writes=[R_pbc[pb]] if k == 0 else [], sig=(k == 7))
                    S.op("dve", lambda e: e.tensor_tensor(
                        out=modbc[:, l, blk * 512:(blk + 1) * 512], in0=pbc[pb][:], in1=bro[wb][:], op=ALU.add),
                        reads=[R_pbc[pb], R_bro[wb]], writes=[R_modbc])
            xc = [0]

            def extract(dst, l, idx, perm):
                xi = xc[0] % 2
                xc[0] += 1
                msk = maskB if perm else maskA
                S.op("pool", lambda e: e.tensor_tensor(out=xtmp[xi][:], in0=modbc[:, l, idx * DM:(idx + 1) * DM], in1=msk[:], op=ALU.mult),
                     reads=[R_modbc, R_msk], writes=[R_xtmp[xi]])
                view = (xtmp[xi][:].rearrange("p (q k) -> p k q", k=8) if perm
                        else xtmp[xi][:].rearrange("p (j q) -> p j q", q=128))
                S.op("dve", lambda e: e.tensor_reduce(out=dst.unsqueeze(2), in_=view, axis=AX.X, op=ALU.add),
                     reads=[R_xtmp[xi]], writes=[R_mod])

            for l in range(2):
                extract(modv[:, l * 48:l * 48 + 8], l, 0, False)
                extract(sct[:], l, 1, False)
                S.op("dve", lambda e: e.scalar_tensor_tensor(
                    out=Av[:, l * 8:(l + 1) * 8], in0=sct[:], scalar=1.0, in1=nrm_sb[:, l * 8:(l + 1) * 8],
                    op0=ALU.add, op1=ALU.mult), reads=[R_mod, R_const], writes=[R_mod])
                extract(modv2[:, l * 16:l * 16 + 8], l, 3, True)
                extract(sct[:], l, 4, True)
                S.op("dve", lambda e: e.scalar_tensor_tensor(
                    out=modv2[:, l * 16 + 8:l * 16 + 16], in0=sct[:], scalar=1.0, in1=nrm2[:, l * 8:(l + 1) * 8],
                    op0=ALU.add, op1=ALU.mult), reads=[R_mod, R_msk], writes=[R_mod])
                for which, idx in (("m", 2), ("f", 5)):
                    gi = gt_idx(l, which)
                    S.op("act", lambda e: e.copy(out=gtbc[:, gi, :], in_=modbc[:, l, idx * DM:(idx + 1) * DM]),
                         reads=[R_modbc], writes=[R_gtbc])
            if dbg:
                d_modv = nc.dram_tensor("d_modv", [128, 96], F32, kind="ExternalOutput").ap()
                d_av = nc.dram_tensor("d_av", [128, 32], F32, kind="ExternalOutput").ap()
                d_gtbc = nc.dram_tensor("d_gtbc", [128, 4 * DM], F32, kind="ExternalOutput").ap()
                S.dma("sp", lambda e: e.dma_start(out=d_modv, in_=modv[:]), reads=[R_mod])
                S.dma("sp", lambda e: e.dma_start(out=d_av, in_=modv2[:]), reads=[R_mod])
                S.dma("sp", lambda e: e.dma_start(out=d_gtbc, in_=gtbc[:].rearrange("p a b -> p (a b)")), reads=[R_gtbc])
            S.barrier()
            S.emit()

        def A_of(l, which):
            i = (0 if which == "m" else 2) + l
            return i * 8

        def norm_transpose(xin, R_xin, xn, R_xn, hT, R_hT, pT, R_pT, junk, R_junk, ss, rstd, R_ss, a_ap, s_ap, perm=False):
            for s in range(4):
                S.op("act", lambda e, s=s: e.activation(out=junk[:], in_=xin[:, s, :], func=AF.Square, accum_out=ss[:, s:s + 1]),
                     reads=[R_xin], writes=[R_junk, R_ss])
            S.op("act", lambda e: e.activation(out=rstd[:], in_=ss[:], func=AF.Sqrt, scale=1.0 / DM, bias=EPS),
                 reads=[R_ss], writes=[R_ss])
            S.op("dve", lambda e: e.reciprocal(out=rstd[:], in_=rstd[:]), reads=[R_ss], writes=[R_ss])
            for s in range(4):
                if s % 2 == 0:
                    S.op("dve", lambda e, s=s: e.tensor_scalar(out=xn[:, s, :], in0=xin[:, s, :], scalar1=rstd[:, s:s + 1],
                                                                scalar2=None, op0=ALU.mult), reads=[R_xin, R_ss], writes=[R_xn])
                else:
                    S.op("act", lambda e, s=s: e.activation(out=xn[:, s, :], in_=xin[:, s, :], func=AF.Identity,
                                                             scale=rstd[:, s:s + 1]), reads=[R_xin, R_ss], writes=[R_xn])
            for k in range(8):
                pb = k % 2
                for s in range(4):
                    src = (xn[:, s, :].rearrange("p (q k) -> p k q", k=8)[:, k, :] if perm else xn[:, s, k * 128:(k + 1) * 128])
                    S.op("pe", lambda e, k=k, s=s, pb=pb, src=src: e.transpose(pT[pb][:, s * 128:(s + 1) * 128], src, ident[:]),
                         reads=[R_xn, R_const], writes=[R_pT[pb]] if s == 0 else [], sig=(s == 3))
                S.op("act", lambda e, k=k, pb=pb: e.activation(out=hT[:, k, :], in_=pT[pb][:, 0:512], func=AF.Identity,
                                                                scale=a_ap[:, k:k + 1], bias=s_ap[:, k:k + 1]),
                     reads=[R_pT[pb], R_mod], writes=[R_hT])

        if nphase >= 1:
            with ExitStack() as st:
                T, P = mkTP(st)

                issue_big_casts()
                wq = T("wq", [128, 8, 2880], BF16)
                cs_sb = T("cs_sb", [128, 48, 16], F32)
                kv_sb = T("kv_sb", [128, 48], F32)
                xin = [T(f"xin{i}", [128, 4, DM], F32) for i in range(2)]
                xn = T("xn", [128, 4, DM], BF16)
                hT = [T(f"hT{i}", [128, 8, 512], BF16) for i in range(2)]
                junk = T("junk", [128, DM], BF16)
                ss = [T(f"ss{i}", [128, 4], F32) for i in range(2)]
                rstd = [T(f"rstd{i}", [128, 4], F32) for i in range(2)]
                qst = [T(f"qst{i}", [128, 960], BF16) for i in range(2)]
                kst = [T(f"kst{i}", [128, 960], BF16) for i in range(2)]
                vst = [T(f"vst{i}", [128, 975], BF16) for i in range(2)]
                tr = [T(f"tr{i}", [128, 4, 8, 8], F32) for i in range(2)]
                pT = [P(f"pT{i}", [128, 1024], BF16) for i in range(2)]
                pb = [P(f"pb{i}", [128, 512], F32) for i in range(6)]
                R_wq, R_cs = Reg("wq"), Reg("cs")
                R_xin = [Reg("xin0"), Reg("xin1")]
                R_xn = Reg("xn")
                R_hT = [Reg("hT0"), Reg("hT1")]
                R_junk = Reg("junk")
                R_ss = [Reg("ss0"), Reg("ss1")]
                R_qst = [Reg("qst0"), Reg("qst1")]
                R_kst = [Reg("kst0"), Reg("kst1")]
                R_vst = [Reg("vst0"), Reg("vst1")]
                R_tr = [Reg("tr0"), Reg("tr1")]
                R_pT = [Reg("pT0"), Reg("pT1")]
                R_pb = [Reg(f"pb{i}") for i in range(6)]

                S.dma("sp", lambda e: e.dma_start(out=cs_sb[:], in_=cs.rearrange("(t p) c -> p t c", p=128)), writes=[R_cs])
                S.dma("sp", lambda e: e.dma_start(out=kv_sb[:], in_=kval), writes=[R_cs])
                for k in range(8):
                    S.dma("sp", lambda e, k=k: e.dma_start(out=wq[:, k, :], in_=wqkv_b[k * 128:(k + 1) * 128, :]),
                          reads=[R_w["wqkv_b"]], writes=[R_wq])

                a0, s0 = A_of(0, "m"), sh_of(0, "m")
                trc = 0
                stc = 0
                import os
                CUT = int(os.environ.get("A1CUT", "9"))
                SKIP = os.environ.get("A1SKIP", "")
                def a1_load(tg):
                    xb = tg % 2
                    S.dma("sp", lambda e: e.dma_start(
                        out=xin[xb][:], in_=xext[tg * 512:(tg + 1) * 512, :].rearrange("(s p) d -> p s d", p=128)),
                        writes=[R_xin[xb]])

                def a1_norm(tg):
                    xb = tg % 2
                    norm_transpose(xin[xb], R_xin[xb], xn, R_xn, hT[xb], R_hT[xb], pT, R_pT, junk, R_junk,
                                   ss[xb], rstd[xb], R_ss[xb], Av[:, 0:8], modv[:, 0:8])

                a1_load(0)
                a1_load(1)
                a1_norm(0)
                for tg in range(12):
                    own = 2 <= tg < 10
                    xb = tg % 2
                    if tg + 2 < 12:
                        a1_load(tg + 2)
                    if tg + 1 < 12:
                        a1_norm(tg + 1)
                    for s in range(4):
                        if CUT < 2:
                            break
                        it = tg * 4 + s
                        sb = stc % 2
                        stc += 1
                        blocks = []
                        if own:
                            blocks += [("q", 0, 512, 0), ("q", 512, 960, 1)]
                        blocks += [("k", 960, 1472, 2), ("k", 1472, 1920, 3), ("v", 1920, 2432, 4), ("v", 2432, 2880, 5)]
                        for kind, c0, c1, bi in blocks:
                            n = c1 - c0
                            for k in range(8):
                                S.op("pe", lambda e, k=k, s=s, c0=c0, c1=c1, n=n, bi=bi, xb=xb: e.matmul(
                                    pb[bi][:, 0:n], lhsT=hT[xb][:, k, s * 128:(s + 1) * 128], rhs=wq[:, k, c0:c1],
                                    start=(k == 0), stop=(k == 7)),
                                    reads=[R_hT[xb], R_wq], writes=[R_pb[bi]] if k == 0 else [], sig=(k == 7))
                            nh = n // 64
                            if CUT < 3:
                                continue
                            if kind in ("q", "k"):
                                stg = qst[sb] if kind == "q" else kst[sb]
                                R_stg = R_qst[sb] if kind == "q" else R_kst[sb]
                                base = c0 if kind == "q" else c0 - 960
                                pv = pb[bi][:, 0:n].rearrange("p (h c) -> p h c", c=64)
                                sv = stg[:, base:base + n].rearrange("p (h c) -> p h c", c=64)
                                ti = trc % 2
                                trc += 1
                                cosb = cs_sb[:, it, 0:8].unsqueeze(1).to_broadcast([128, nh, 8])
                                sinb = cs_sb[:, it, 8:16].unsqueeze(1).to_broadcast([128, nh, 8])
                                if "a" not in SKIP:
                                    S.op("act", lambda e, pv=pv, sv=sv: e.copy(out=sv[:, :, 16:64], in_=pv[:, :, 16:64]),
                                         reads=[R_pb[bi]], writes=[R_stg])
                                for ti2, (a, b) in enumerate((((0, 8), cosb), ((8, 16), sinb), ((8, 16), cosb), ((0, 8), sinb))):
                                    if "b" in SKIP:
                                        continue
                                    S.op("dve", lambda e, pv=pv, a=a, b=b, ti=ti, ti2=ti2, nh=nh: e.tensor_tensor(
                                        out=tr[ti][:, ti2, 0:nh, :], in0=pv[:, :, a[0]:a[1]], in1=b, op=ALU.mult),
                                        reads=[R_pb[bi], R_cs], writes=[R_tr[ti]])
                                if "c" not in SKIP:
                                  S.op("pool", lambda e, sv=sv, ti=ti, nh=nh: e.tensor_tensor(
                                    out=sv[:, :, 0:8], in0=tr[ti][:, 0, 0:nh, :], in1=tr[ti][:, 1, 0:nh, :], op=ALU.subtract),
                                    reads=[R_tr[ti]], writes=[R_stg])
                                if "c" not in SKIP:
                                  S.op("pool", lambda e, sv=sv, ti=ti, nh=nh: e.tensor_tensor(
                                    out=sv[:, :, 8:16], in0=tr[ti][:, 2, 0:nh, :], in1=tr[ti][:, 3, 0:nh, :], op=ALU.add),
                                    reads=[R_tr[ti]], writes=[R_stg])
                            else:
                                h0 = (c0 - 1920) // 64
                                pv = pb[bi][:, 0:n].rearrange("p (h c) -> p h c", c=64)
                                vv = vst[sb][:].rearrange("p (h c) -> p h c", c=65)
                                if "d" not in SKIP:
                                  S.op("act", lambda e, pv=pv, vv=vv, h0=h0, nh=nh, it=it: e.activation(
                                    out=vv[:, h0:h0 + nh, 0:64], in_=pv, func=AF.Identity, scale=kv_sb[:, it:it + 1]),
                                    reads=[R_pb[bi], R_cs], writes=[R_vst[sb]])
                                if bi == 5 and "e" not in SKIP:
                                    S.op("pool", lambda e, vv=vv, it=it: e.tensor_copy(
                                        out=vv[:, :, 64:65], in_=kv_sb[:, it:it + 1].unsqueeze(1).to_broadcast([128, 15, 1])),
                                        reads=[R_cs], writes=[R_vst[sb]])
                        if CUT < 4:
                            continue
                        if own:
                            ot = it - 8
                            S.dma("sp", lambda e, ot=ot, sb=sb: e.dma_start(out=q_s[ot * 128:(ot + 1) * 128, :], in_=qst[sb][:]),
                                  reads=[R_qst[sb]], writes=[])
                        S.dma("sp", lambda e, it=it, sb=sb: e.dma_start(out=k_s[it * 128:(it + 1) * 128, :], in_=kst[sb][:]),
                              reads=[R_kst[sb]], writes=[])
                        S.dma("sp", lambda e, it=it, sb=sb: e.dma_start(out=v_s[it * 128:(it + 1) * 128, :], in_=vst[sb][:]),
                              reads=[R_vst[sb]], writes=[])
                S.barrier()
                S.emit()

        if nphase >= 1 and asub >= 2:
            with ExitStack() as st:
                T, P = mkTP(st)

                UB = 8
                band = T("band", [128, 256], BF16)
                Qt = [T(f"Qt{i}", [128, UB, 384], BF16) for i in range(2)]
                Kt = [T(f"Kt{i}", [128, UB + 1, 384], BF16) for i in range(2)]
                Vt = [T(f"Vt{i}", [128, UB + 1, 325], BF16) for i in range(2)]
                qT = [T(f"qT{i}", [128, 3, UB * 128], BF16) for i in range(2)]
                kT = [T(f"kT{i}", [128, 3, (UB + 1) * 128], BF16) for i in range(2)]
                NPS = 3
                Pe = [T(f"Pe{i}", [128, 5, 256], BF16) for i in range(NPS)]
                PT = [T(f"PT{i}", [128, 5, 256], BF16) for i in range(NPS)]
                Oev = [T(f"Oev{i}", [128, 325], F32) for i in range(3)]
                NSB = 4
                pTr = [P(f"pTr{i}", [128, 1024], BF16) for i in range(2)]
                pS = [P(f"pS{i}", [128, 512], F32) for i in range(NSB)]
                pO = [P(f"pO{i}", [128, 512], F32) for i in range(2)]
                R_band = Reg("band")
                R_Qt = [Reg("Qt0"), Reg("Qt1")]
                R_Kt = [Reg("Kt0"), Reg("Kt1")]
                R_Vt = [Reg("Vt0"), Reg("Vt1")]
                R_qT = [Reg("qT0"), Reg("qT1")]
                R_kT = [Reg("kT0"), Reg("kT1")]
                R_Pe = [Reg(f"Pe{i}") for i in range(NPS)]
                R_PT = [Reg(f"PT{i}") for i in range(NPS)]
                R_Oev = [Reg(f"Oev{i}") for i in range(3)]
                R_pTr = [Reg("pTr0"), Reg("pTr1")]
                R_pS = [Reg(f"pS{i}") for i in range(NSB)]
                R_pO = [Reg("pO0"), Reg("pO1")]
                S.dma("sp", lambda e: e.dma_start(out=band[:], in_=band_d), writes=[R_band])
                for i in range(2):
                    S.op("pool", lambda e, i=i: e.memset(Qt[i][:, :, 320:384], 0.0), writes=[R_Qt[i]])
                    S.op("pool", lambda e, i=i: e.memset(Kt[i][:, :, 320:384], 0.0), writes=[R_Kt[i]])

                units = []
                for g, d in enumerate((1, 4, 16)):
                    nbt = 32 // d
                    ub = min(UB, nbt)
                    for r in range(d):
                        for n0 in range(0, nbt, ub):
                            units.append((g, d, r, n0, ub))

                def load_qk(u, ub_i):
                    g, d, r, n0, nb = u
                    qv = q_s.rearrange("(i d) c -> d i c", d=d)
                    kv = k_s.rearrange("(i d) c -> d i c", d=d)
                    i0 = n0 * 128 - 64 + HALO // d
                    S.dma("sp", lambda e: e.dma_start(
                        out=Qt[ub_i][:, 0:nb, 0:320],
                        in_=qv[r, n0 * 128:(n0 + nb) * 128, g * 320:(g + 1) * 320].rearrange("(n p) c -> p n c", p=128)),
                        reads=[R_q], writes=[R_Qt[ub_i]])
                    S.dma("sp", lambda e: e.dma_start(
                        out=Kt[ub_i][:, 0:nb + 1, 0:320],
                        in_=kv[r, i0:i0 + (nb + 1) * 128, g * 320:(g + 1) * 320].rearrange("(n p) c -> p n c", p=128)),
                        reads=[R_k], writes=[R_Kt[ub_i]])

                def load_v(u, ub_i):
                    g, d, r, n0, nb = u
                    vv = v_s.rearrange("(i d) c -> d i c", d=d)
                    i0 = n0 * 128 - 64 + HALO // d
                    S.dma("sp", lambda e: e.dma_start(
                        out=Vt[ub_i][:, 0:nb + 1, :],
                        in_=vv[r, i0:i0 + (nb + 1) * 128, g * 325:(g + 1) * 325].rearrange("(n p) c -> p n c", p=128)),
                        reads=[R_v], writes=[R_Vt[ub_i]])

                cnt = {"tr": 0, "ps": 0, "pe": 0, "oe": 0}

                def make_tasks(u, bi):
                    g, d, r, n0, nb = u
                    tasks = []
                    for src, R_src, dst, R_dst, cnt_n in ((Qt[bi], R_Qt[bi], qT[bi], R_qT[bi], nb),
                                                            (Kt[bi], R_Kt[bi], kT[bi], R_kT[bi], nb + 1)):
                        for n in range(cnt_n):
                            def task(src=src, R_src=R_src, dst=dst, R_dst=R_dst, n=n):
                                tb = cnt["tr"] % 2
                                cnt["tr"] += 1
                                for pr in range(3):
                                    S.op("pe", lambda e: e.transpose(
                                        pTr[tb][:, pr * 128:(pr + 1) * 128], src[:, n, pr * 128:(pr + 1) * 128], ident[:]),
                                        reads=[R_src, R_const], writes=[R_pTr[tb]] if pr == 0 else [], sig=(pr == 2))
                                if tb == 0:
                                    S.op("act", lambda e: e.copy(
                                        out=dst[:, 0:2, n * 128:(n + 1) * 128],
                                        in_=pTr[tb][:, 0:256].rearrange("p (a c) -> p a c", c=128)), reads=[R_pTr[tb]], writes=[R_dst])
                                    S.op("act", lambda e: e.copy(
                                        out=dst[0:64, 2, n * 128:(n + 1) * 128], in_=pTr[tb][0:64, 256:384]),
                                        reads=[R_pTr[tb]], writes=[R_dst])
                                else:
                                    S.op("dve", lambda e: e.tensor_copy(
                                        out=dst[:, 0:2, n * 128:(n + 1) * 128],
                                        in_=pTr[tb][:, 0:256].rearrange("p (a c) -> p a c", c=128)), reads=[R_pTr[tb]], writes=[R_dst])
                                    S.op("dve", lambda e: e.tensor_copy(
                                        out=dst[0:64, 2, n * 128:(n + 1) * 128], in_=pTr[tb][0:64, 256:384]),
                                        reads=[R_pTr[tb]], writes=[R_dst])
                            tasks.append(task)
                    return tasks

                def s_stage(u, bi, j):
                    g, d, r, n0, nb = u
                    c0 = 0 if j >= 1 else 128
                    c1 = 256 if j < nb else 128
                    q0 = (j - 1) * 128 + c0
                    wq_ = c1 - c0
                    pi = cnt["pe"] % NPS
                    cnt["pe"] += 1
                    for bk, hs in enumerate(((0, 2), (1, 3), (4,))):
                        sbk = cnt["ps"] % NSB
                        cnt["ps"] += 1
                        for hh, h in enumerate(hs):
                            pr, base = h // 2, (h % 2) * 64
                            S.op("pe", lambda e: e.matmul(
                                pS[sbk][:, hh * 256 + c0:hh * 256 + c1],
                                lhsT=kT[bi][base:base + 64, pr, j * 128:(j + 1) * 128],
                                rhs=qT[bi][base:base + 64, pr, q0:q0 + wq_], start=True, stop=True),
                                reads=[R_kT[bi], R_qT[bi]], writes=[R_pS[sbk]] if hh == 0 else [],
                                sig=(hh == len(hs) - 1))
                        nhb = len(hs)
                        S.op("act", lambda e: e.activation(
                            out=Pe[pi][:, bk * 2:bk * 2 + nhb, c0:c1],
                            in_=pS[sbk][:, 0:nhb * 256].rearrange("p (h c) -> p h c", c=256)[:, :, c0:c1],
                            func=AF.Exp, scale=0.125), reads=[R_pS[sbk]], writes=[R_Pe[pi]])
                    S.op("dve", lambda e: e.tensor_tensor(
                        out=PT[pi][:, :, c0:c1], in0=Pe[pi][:, :, c0:c1],
                        in1=band[:, c0:c1].unsqueeze(1).to_broadcast([128, 5, c1 - c0]), op=ALU.mult),
                        reads=[R_Pe[pi], R_band], writes=[R_PT[pi]])
                    return pi

                def pv_stage(u, bi, j, pi):
                    g, d, r, n0, nb = u
                    for n in (j - 1, j):
                        if n < 0 or n >= nb:
                            continue
                        cc = 0 if n == j - 1 else 128
                        ob = n % 2
                        for h in range(5):
                            sl = (0, 2, 1, 3, 4)[h]
                            S.op("pe", lambda e: e.matmul(
                                pO[ob][:, h * 65:(h + 1) * 65], lhsT=PT[pi][:, sl, cc:cc + 128],
                                rhs=Vt[bi][:, j, h * 65:(h + 1) * 65], start=(j == n and h == 0), stop=(j == n + 1),
                                skip_group_check=True),
                                reads=[R_PT[pi], R_Vt[bi]], writes=[R_pO[ob]] if h == 0 else [], sig=(h == 4))
                        if j == n + 1:
                            oi = cnt["oe"] % 3
                            cnt["oe"] += 1
                            if cnt["oe"] % 2 == 0:
                                S.op("act", lambda e: e.copy(out=Oev[oi][:], in_=pO[ob][:, 0:325]),
                                     reads=[R_pO[ob]], writes=[R_Oev[oi]])
                            else:
                                S.op("dve", lambda e: e.tensor_copy(out=Oev[oi][:], in_=pO[ob][:, 0:325]),
                                     reads=[R_pO[ob]], writes=[R_Oev[oi]])
                            ov = o_s.rearrange("(i d) c -> d i c", d=d)
                            nn = n0 + n
                            S.dma("sp", lambda e: e.dma_start(
                                out=ov[r, nn * 128:(nn + 1) * 128, g * 325:(g + 1) * 325], in_=Oev[oi][:]),
                                reads=[R_Oev[oi]], writes=[])

                NU = len(units)
                load_qk(units[0], 0)
                load_v(units[0], 0)
                if NU > 1:
                    load_qk(units[1], 1)
                for tk in make_tasks(units[0], 0):
                    tk()
                for ui, u in enumerate(units):
                    bi = ui % 2
                    nb = u[4]
                    if ui + 2 < NU:
                        load_qk(units[ui + 2], bi)
                    if ui + 1 < NU:
                        load_v(units[ui + 1], 1 - bi)
                    tasks = make_tasks(units[ui + 1], 1 - bi) if ui + 1 < NU else []
                    per = -(-len(tasks) // (nb + 1)) if tasks else 0
                    prev = None
                    for j in range(nb + 1):
                        pi = s_stage(u, bi, j)
                        for _ in range(per):
                            if tasks:
                                tasks.pop(0)()
                        if prev is not None:
                            pv_stage(u, bi, prev[0], prev[1])
                        prev = (j, pi)
                    pv_stage(u, bi, prev[0], prev[1])
                    while tasks:
                        tasks.pop(0)()
                S.barrier()
                S.emit()

        if nphase >= 1 and asub >= 3:
            with ExitStack() as st:
                T, P = mkTP(st)

                wo_sb = T("wo_sb", [128, 8, DM], BF16)
                Ot = [T(f"Ot{i}", [128, 975], F32) for i in range(2)]
                xt = [T(f"xt{i}", [128, DM], F32) for i in range(3)]
                Dn = [T(f"Dn{i}", [128, 5], F32) for i in range(2)]
                ob = [T(f"ob{i}", [128, 1024], BF16) for i in range(2)]
                oT = [T(f"oT{i}", [128, 8, 128], BF16) for i in range(2)]
                tmp = [T(f"tmp{i}", [128, 512], F32) for i in range(2)]
                pTr = [P(f"pTr{i}", [128, 1024], BF16) for i in range(2)]
                py = [P(f"py{i}", [128, 512], F32) for i in range(4)]
                R_wo = Reg("wo")
                R_Ot = [Reg("Ot0"), Reg("Ot1")]
                R_xt = [Reg("xt0"), Reg("xt1"), Reg("xt2")]
                R_Dn = [Reg("Dn0"), Reg("Dn1")]
                R_ob = [Reg("ob0"), Reg("ob1")]
                R_oT = [Reg("oT0"), Reg("oT1")]
                R_tmp = [Reg("tmp0"), Reg("tmp1")]
                R_pTr = [Reg("pTr0"), Reg("pTr1")]
                R_py = [Reg(f"py{i}") for i in range(4)]
                S.dma("sp", lambda e: e.dma_start(out=wo_sb[:, 0:7, :], in_=wo_b[0:896, :].rearrange("(k p) n -> p k n", p=128)),
                      reads=[R_w["wo_b"]], writes=[R_wo])
                S.dma("sp", lambda e: e.dma_start(out=wo_sb[0:64, 7, :], in_=wo_b[896:960, :]), reads=[R_w["wo_b"]], writes=[R_wo])
                gi = gt_idx(0, "m")
                tmc = 0
                for i in range(2):
                    S.op("pool", lambda e, i=i: e.memset(ob[i][:, 960:1024], 0.0), writes=[R_ob[i]])
                tmc3 = [0]

                def a3_loads(t):
                    b = t % 2
                    xb3 = t % 3
                    S.dma("sp", lambda e: e.dma_start(out=Ot[b][:], in_=o_s[t * 128:(t + 1) * 128, :]),
                          reads=[R_o], writes=[R_Ot[b]])
                    S.dma("sp", lambda e: e.dma_start(out=xt[xb3][:], in_=xext[HALO + t * 128:HALO + (t + 1) * 128, :]),
                          writes=[R_xt[xb3]])

                def a3_stage1(t):
                    b = t % 2
                    O4 = Ot[b][:].rearrange("p (g h c) -> p g h c", g=3, h=5)
                    S.op("dve", lambda e, O4=O4, b=b: e.tensor_tensor(out=Dn[b][:], in0=O4[:, 0, :, 64], in1=O4[:, 1, :, 64], op=ALU.add),
                         reads=[R_Ot[b]], writes=[R_Dn[b]])
                    S.op("dve", lambda e, O4=O4, b=b: e.tensor_tensor(out=Dn[b][:], in0=Dn[b][:], in1=O4[:, 2, :, 64], op=ALU.add),
                         reads=[R_Ot[b], R_Dn[b]], writes=[R_Dn[b]])
                    S.op("dve", lambda e, b=b: e.reciprocal(out=Dn[b][:], in_=Dn[b][:]), reads=[R_Dn[b]], writes=[R_Dn[b]])
                    ob4 = ob[b][:, 0:960].rearrange("p (g h c) -> p g h c", g=3, h=5)
                    for g in range(3):
                        eng = "pool" if g < 2 else "dve"
                        S.op(eng, lambda e, g=g, O4=O4, ob4=ob4, b=b: e.tensor_tensor(
                            out=ob4[:, g], in0=O4[:, g, :, 0:64], in1=Dn[b][:].unsqueeze(2).to_broadcast([128, 5, 64]), op=ALU.mult),
                            reads=[R_Ot[b], R_Dn[b]], writes=[R_ob[b]])

                def a3_stage2(t):
                    b = t % 2
                    xb3 = t % 3
                    for kc in range(8):
                        w = 128
                        S.op("pe", lambda e, kc=kc, w=w, b=b: e.transpose(
                            pTr[b][0:w, kc * 128:(kc + 1) * 128], ob[b][:, kc * 128:kc * 128 + w], ident[:]),
                            reads=[R_ob[b], R_const], writes=[R_pTr[b]] if kc == 0 else [], sig=(kc == 7))
                    S.op("act", lambda e, b=b: e.copy(out=oT[b][:, 0:7, :], in_=pTr[b][:, 0:896].rearrange("p (k c) -> p k c", c=128)),
                         reads=[R_pTr[b]], writes=[R_oT[b]])
                    S.op("act", lambda e, b=b: e.copy(out=oT[b][0:64, 7, :], in_=pTr[b][0:64, 896:1024]),
                         reads=[R_pTr[b]], writes=[R_oT[b]])
                    for nbk in range(2):
                        yb = (t * 2 + nbk) % 4
                        for kc in range(8):
                            w = 128 if kc < 7 else 64
                            S.op("pe", lambda e, kc=kc, w=w, nbk=nbk, yb=yb, b=b: e.matmul(
                                py[yb][:], lhsT=oT[b][0:w, kc, :], rhs=wo_sb[0:w, kc, nbk * 512:(nbk + 1) * 512],
                                start=(kc == 0), stop=(kc == 7)),
                                reads=[R_oT[b], R_wo], writes=[R_py[yb]] if kc == 0 else [], sig=(kc == 7))
                        ti = tmc3[0] % 2
                        tmc3[0] += 1
                        S.op("dve", lambda e, yb=yb, ti=ti, nbk=nbk: e.tensor_tensor(
                            out=tmp[ti][:], in0=py[yb][:], in1=gtbc[:, gi, nbk * 512:(nbk + 1) * 512], op=ALU.mult),
                            reads=[R_py[yb], R_gtbc], writes=[R_tmp[ti]])
                        S.op("pool", lambda e, ti=ti, nbk=nbk, b=b: e.tensor_tensor(
                            out=xt[xb3][:, nbk * 512:(nbk + 1) * 512], in0=xt[xb3][:, nbk * 512:(nbk + 1) * 512], in1=tmp[ti][:], op=ALU.add),
                            reads=[R_tmp[ti], R_xt[xb3]], writes=[R_xt[xb3]])
                    S.dma("sp", lambda e, t=t, b=b: e.dma_start(out=resid[t * 128:(t + 1) * 128, :], in_=xt[xb3][:]),
                          reads=[R_xt[xb3]], writes=[R_res[t // 4]])

                a3_loads(0)
                a3_loads(1)
                a3_stage1(0)
                for t in range(32):
                    if t + 2 < 32:
                        a3_loads(t + 2)
                    if t + 1 < 32:
                        a3_stage1(t + 1)
                    a3_stage2(t)
                S.barrier()
                S.emit()

        def moe_phase(l, final):
            with ExitStack() as st:
                T, P = mkTP(st)

                wr_f = T("wr_f", [128, 8, 20], F32)
                wr_sb = T("wr_sb", [128, 8, 20], BF16)
                br_sb = T("br_sb", [128, 40], F32)
                xt = [T(f"xt{i}", [128, 4, DM], F32) for i in range(2)]
                xn = T("xn", [128, 4, DM], BF16)
                hT = [T(f"hT{i}", [128, 8, 512], BF16) for i in range(2)]
                junk = T("junk", [128, DM], BF16)
                ss = [T(f"ss{i}", [128, 4], F32) for i in range(2)]
                rstd = [T(f"rstd{i}", [128, 4], F32) for i in range(2)]
                actT = T("actT", [128, 32, 512], BF16)
                Gb = [T("Gb0", [128, 4, 16, 128], BF16)] * 2
                NWG = 3
                wgu = [T(f"wgu{i}", [128, 8, 512], BF16) for i in range(NWG)]
                NWD = 6
                wd = [T(f"wd{i}", [128, 2, 512], BF16) for i in range(NWD)]
                sg = [T(f"sg{i}", [128, 512], F32) for i in range(2)]
                tg_ = [T(f"tg{i}", [128, 512], F32) for i in range(2)]
                tmp = [T(f"tmp{i}", [128, 512], F32) for i in range(2)]
                rt = T("rt", [128, 4, 96], F32)
                gate = T("gate", [128, 4, 16], F32)
                pmain = [P(f"pm{i}", [128, 512], F32) for i in range(6)]
                pT = [P(f"pT{i}", [128, 1024], BF16) for i in range(1)]
                pR = P("pR", [128, 512], F32)
                R_wr = Reg("wr")
                R_xt = [Reg("xt0"), Reg("xt1")]
                R_xn = Reg("xn")
                R_hT = [Reg("hT0"), Reg("hT1")]
                R_junk = Reg("junk")
                R_ss = [Reg("ss0"), Reg("ss1")]
                R_act = [Reg(f"act{c}") for c in range(32)]
                R_Gb = [Reg("Gb0")] * 2
                R_wgu = [Reg(f"wgu{i}") for i in range(NWG)]
                R_wd = [Reg(f"wd{i}") for i in range(NWD)]
                R_sg = [Reg("sg0"), Reg("sg1")]
                R_tg = [Reg("tg0"), Reg("tg1")]
                R_tmp = [Reg("tmp0"), Reg("tmp1")]
                R_rt = Reg("rt")
                R_gate = Reg("gate")
                R_pm = [Reg(f"pm{i}") for i in range(6)]
                R_pT = [Reg("pT0")]
                R_pR = Reg("pR")

                S.dma("sp", lambda e: e.dma_start(out=wr_f[:], in_=wr[l].rearrange("(p k) n -> p k n", k=8)), writes=[R_wr])
                S.dma("sp", lambda e: e.dma_start(out=br_sb[:], in_=brr.partition_broadcast(128)), writes=[R_wr])
                S.op("dve", lambda e: e.tensor_copy(out=wr_sb[:], in_=wr_f[:]), reads=[R_wr], writes=[R_wr])
                a0, s0 = A_of(l, "f"), sh_of(l, "f")
                gi = gt_idx(l, "f")
                R_eg, R_eu, R_ed = R_w[f"eg{l}"], R_w[f"eu{l}"], R_w[f"ed{l}"]
                wgc = 0
                wdc = 0
                sgc = 0
                tmc = 0

                def load_x(t):
                    b = t % 2
                    S.dma("sp", lambda e: e.dma_start(
                        out=xt[b][:], in_=resid[t * 512:(t + 1) * 512, :].rearrange("(s p) d -> p s d", p=128)),
                        reads=[R_res[t]], writes=[R_xt[b]])

                gu_next = [0]
                d_next = [0]

                def issue_gu(upto):
                    while gu_next[0] < min(upto, 128):
                        i = gu_next[0]
                        gu_next[0] += 1
                        ex, wi = i % 16, i % NWG
                        S.dma("sp", lambda e: e.dma_start(
                            out=wgu[wi][:, :, 0:256], in_=eg_b[l, ex].rearrange("(p k) f -> p k f", k=8)),
                            reads=[R_eg], writes=[R_wgu[wi]])
                        S.dma("sp", lambda e: e.dma_start(
                            out=wgu[wi][:, :, 256:512], in_=eu_b[l, ex].rearrange("(p k) f -> p k f", k=8)),
                            reads=[R_eu], writes=[R_wgu[wi]])

                def issue_d(upto):
                    while d_next[0] < min(upto, 256):
                        i = d_next[0]
                        d_next[0] += 1
                        ex, nbk, di = i % 16, (i // 16) % 2, i % NWD
                        S.dma("sp", lambda e: e.dma_start(
                            out=wd[di][:], in_=ed_b[l, ex, :, nbk * 512:(nbk + 1) * 512].rearrange("(c p) d -> p c d", p=128)),
                            reads=[R_ed], writes=[R_wd[di]])

                def norm_router(t):
                    b = t % 2
                    norm_transpose(xt[b], R_xt[b], xn, R_xn, hT[b], R_hT[b], [pT[0], pT[0]], [R_pT[0], R_pT[0]],
                                   junk, R_junk, ss[b], rstd[b], R_ss[b],
                                   modv2[:, l * 16 + 8:l * 16 + 16], modv2[:, l * 16:l * 16 + 8], perm=True)
                    for s in range(4):
                        for k in range(8):
                            S.op("pe", lambda e, s=s, k=k, b=b: e.matmul(
                                pR[:, s * 32:s * 32 + 20], lhsT=hT[b][:, k, s * 128:(s + 1) * 128], rhs=wr_sb[:, k, :],
                                start=(k == 0), stop=(k == 7)),
                                reads=[R_hT[b], R_wr], writes=[R_pR] if k == 0 else [], sig=(k == 7))
                    lg = rt[:, :, 0:20]
                    S.op("dve", lambda e: e.tensor_tensor(
                        out=lg, in0=pR[:, 0:128].rearrange("p (s c) -> p s c", c=32)[:, :, 0:20],
                        in1=br_sb[:, l * 20:(l + 1) * 20].unsqueeze(1).to_broadcast([128, 4, 20]), op=ALU.add),
                        reads=[R_pR, R_wr], writes=[R_rt])
                    gl = rt[:, :, 0:4]
                    el = rt[:, :, 4:20]
                    gmx = rt[:, :, 20:21]
                    S.op("dve", lambda e: e.tensor_reduce(out=rt[:, :, 20:21], in_=gl, axis=AX.X, op=ALU.max), reads=[R_rt], writes=[R_rt])
                    ohg = rt[:, :, 24:28]
                    S.op("dve", lambda e: e.tensor_tensor(out=ohg, in0=gl, in1=gmx.to_broadcast([128, 4, 4]), op=ALU.is_equal),
                         reads=[R_rt], writes=[R_rt])
                    gsh = rt[:, :, 28:32]
                    S.op("dve", lambda e: e.tensor_tensor(out=gsh, in0=gl, in1=gmx.to_broadcast([128, 4, 4]), op=ALU.subtract),
                         reads=[R_rt], writes=[R_rt])
                    S.op("act", lambda e: e.activation(out=gsh, in_=gsh, func=AF.Exp), reads=[R_rt], writes=[R_rt])
                    S.op("dve", lambda e: e.tensor_reduce(out=rt[:, :, 21:22], in_=gsh, axis=AX.X, op=ALU.add), reads=[R_rt], writes=[R_rt])
                    S.op("dve", lambda e: e.reciprocal(out=rt[:, :, 21:22], in_=rt[:, :, 21:22]), reads=[R_rt], writes=[R_rt])
                    msk = rt[:, :, 32:48]
                    S.op("dve", lambda e: e.tensor_scalar(
                        out=msk.rearrange("p s (g x) -> p s g x", x=4), in0=ohg.unsqueeze(3).to_broadcast([128, 4, 4, 4]),
                        scalar1=-1.0, scalar2=BIG, op0=ALU.add, op1=ALU.mult), reads=[R_rt], writes=[R_rt])
                    elm = rt[:, :, 48:64]
                    S.op("dve", lambda e: e.tensor_tensor(out=elm, in0=el, in1=msk, op=ALU.add), reads=[R_rt], writes=[R_rt])
                    S.op("dve", lambda e: e.tensor_reduce(out=rt[:, :, 22:23], in_=elm, axis=AX.X, op=ALU.max), reads=[R_rt], writes=[R_rt])
                    oh1 = rt[:, :, 64:80]
                    S.op("dve", lambda e: e.tensor_tensor(out=oh1, in0=elm, in1=rt[:, :, 22:23].to_broadcast([128, 4, 16]), op=ALU.is_equal),
                         reads=[R_rt], writes=[R_rt])
                    elm2 = rt[:, :, 32:48]
                    S.op("dve", lambda e: e.scalar_tensor_tensor(out=elm2, in0=oh1, scalar=-BIG, in1=elm, op0=ALU.mult, op1=ALU.add),
                         reads=[R_rt], writes=[R_rt])
                    S.op("dve", lambda e: e.tensor_reduce(out=rt[:, :, 23:24], in_=elm2, axis=AX.X, op=ALU.max), reads=[R_rt], writes=[R_rt])
                    oh2 = rt[:, :, 80:96]
                    S.op("dve", lambda e: e.tensor_tensor(out=oh2, in0=elm2, in1=rt[:, :, 23:24].to_broadcast([128, 4, 16]), op=ALU.is_equal),
                         reads=[R_rt], writes=[R_rt])
                    dd = rt[:, :, 28:29]
                    S.op("dve", lambda e: e.tensor_tensor(out=dd, in0=rt[:, :, 23:24], in1=rt[:, :, 22:23], op=ALU.subtract),
                         reads=[R_rt], writes=[R_rt])
                    S.op("act", lambda e: e.activation(out=dd, in_=dd, func=AF.Exp), reads=[R_rt], writes=[R_rt])
                    p1 = rt[:, :, 29:30]
                    S.op("dve", lambda e: e.tensor_scalar(out=p1, in0=dd, scalar1=1.0, scalar2=None, op0=ALU.add), reads=[R_rt], writes=[R_rt])
                    S.op("dve", lambda e: e.reciprocal(out=p1, in_=p1), reads=[R_rt], writes=[R_rt])
                    p2 = rt[:, :, 30:31]
                    S.op("dve", lambda e: e.tensor_tensor(out=p2, in0=dd, in1=p1, op=ALU.mult), reads=[R_rt], writes=[R_rt])
                    S.op("dve", lambda e: e.tensor_tensor(out=p1, in0=p1, in1=rt[:, :, 21:22], op=ALU.mult), reads=[R_rt], writes=[R_rt])
                    S.op("dve", lambda e: e.tensor_tensor(out=p2, in0=p2, in1=rt[:, :, 21:22], op=ALU.mult), reads=[R_rt], writes=[R_rt])
                    S.op("dve", lambda e: e.tensor_tensor(out=oh1, in0=oh1, in1=p1.to_broadcast([128, 4, 16]), op=ALU.mult),
                         reads=[R_rt], writes=[R_rt])
                    S.op("dve", lambda e: e.tensor_tensor(out=oh2, in0=oh2, in1=p2.to_broadcast([128, 4, 16]), op=ALU.mult),
                         reads=[R_rt], writes=[R_rt])
                    S.op("dve", lambda e: e.tensor_tensor(out=gate[:], in0=oh1, in1=oh2, op=ALU.add), reads=[R_rt], writes=[R_gate])
                    S.op("pool", lambda e, b=b: e.tensor_copy(out=Gb[b][:], in_=gate[:].unsqueeze(3).to_broadcast([128, 4, 16, 128])),
                         reads=[R_gate], writes=[R_Gb[b]])

                def experts(t, hook):
                    b = t % 2
                    for ex in range(16):
                        gi_ = t * 16 + ex
                        wi = gi_ % NWG
                        issue_gu(gi_ + 1)
                        pgb = 4 + ex % 2
                        for s in range(4):
                            S.op("pe", lambda e, s=s, ex=ex, pgb=pgb, b=b: e.matmul(
                                pmain[pgb][:, s * 128:(s + 1) * 128], lhsT=Gb[b][:, s, ex, :], rhs=ident[:], start=True, stop=True),
                                reads=[R_Gb[b], R_const], writes=[R_pm[pgb]] if s == 0 else [], sig=(s == 3))
                        for fc in range(2):
                            c = ex * 2 + fc
                            hb = c % 2
                            for which, pbk in (("g", hb), ("u", 2 + hb)):
                                off = fc * 128 + (256 if which == "u" else 0)
                                for k in range(8):
                                    S.op("pe", lambda e, k=k, off=off, pbk=pbk, wi=wi, b=b: e.matmul(
                                        pmain[pbk][:], lhsT=wgu[wi][:, k, off:off + 128], rhs=hT[b][:, k, :],
                                        start=(k == 0), stop=(k == 7)),
                                        reads=[R_wgu[wi], R_hT[b]], writes=[R_pm[pbk]] if k == 0 else [], sig=(k == 7))
                            si = sgc_[0] % 2
                            sgc_[0] += 1
                            S.op("act", lambda e, hb=hb, si=si: e.activation(out=sg[si][:], in_=pmain[hb][:], func=AF.Silu),
                                 reads=[R_pm[hb]], writes=[R_sg[si]])
                            S.op("dve", lambda e, si=si, pgb=pgb: e.tensor_tensor(out=tg_[si][:], in0=sg[si][:], in1=pmain[pgb][:], op=ALU.mult),
                                 reads=[R_sg[si], R_pm[pgb]], writes=[R_tg[si]])
                            S.op("dve", lambda e, si=si, hb=hb, c=c: e.tensor_tensor(out=actT[:, c, :], in0=tg_[si][:], in1=pmain[2 + hb][:], op=ALU.mult),
                                 reads=[R_tg[si], R_pm[2 + hb]], writes=[R_act[c]])
                        issue_gu(gi_ + NWG + 1)
                        if ex == 2:
                            hook()

                def down(t):
                    b = t % 2
                    for nbk in range(2):
                        for ex in range(16):
                            di_ = t * 32 + nbk * 16 + ex
                            di = di_ % NWD
                            issue_d(di_ + 1)
                            for fc in range(2):
                                c = ex * 2 + fc
                                for s in range(4):
                                    S.op("pe", lambda e, c=c, s=s, di=di, fc=fc: e.matmul(
                                        pmain[s][:], lhsT=actT[:, c, s * 128:(s + 1) * 128], rhs=wd[di][:, fc, :],
                                        start=(c == 0), stop=(c == 31)),
                                        reads=[R_act[c], R_wd[di]], writes=[R_pm[s]] if c in (0, 31) else [], sig=(c == 31 or s == 3))
                            issue_d(di_ + NWD + 1)
                        for s in range(4):
                            ti = tmc_[0] % 2
                            tmc_[0] += 1
                            S.op("dve", lambda e, s=s, ti=ti, nbk=nbk: e.tensor_tensor(
                                out=tmp[ti][:], in0=pmain[s][:], in1=gtbc[:, gi, nbk * 512:(nbk + 1) * 512], op=ALU.mult),
                                reads=[R_pm[s], R_gtbc], writes=[R_tmp[ti]])
                            S.op("pool", lambda e, s=s, ti=ti, nbk=nbk, b=b: e.tensor_tensor(
                                out=xt[b][:, s, nbk * 512:(nbk + 1) * 512], in0=xt[b][:, s, nbk * 512:(nbk + 1) * 512],
                                in1=tmp[ti][:], op=ALU.add), reads=[R_tmp[ti], R_xt[b]], writes=[R_xt[b]])

                def finish(t):
                    b = t % 2
                    if not final:
                        return lambda: S.dma("sp", lambda e: e.dma_start(
                            out=resid[t * 512:(t + 1) * 512, :].rearrange("(s p) d -> p s d", p=128), in_=xt[b][:]),
                            reads=[R_xt[b]], writes=[R_res[t]])
                    else:
                        for s in range(4):
                            S.op("act", lambda e, s=s, b=b: e.activation(out=junk[:], in_=xt[b][:, s, :], func=AF.Square,
                                                                          accum_out=ss[b][:, s:s + 1]),
                                 reads=[R_xt[b]], writes=[R_junk, R_ss[b]])
                        S.op("act", lambda e, b=b: e.activation(out=rstd[b][:], in_=ss[b][:], func=AF.Sqrt, scale=1.0 / DM, bias=EPS),
                             reads=[R_ss[b]], writes=[R_ss[b]])
                        S.op("dve", lambda e, b=b: e.reciprocal(out=rstd[b][:], in_=rstd[b][:]), reads=[R_ss[b]], writes=[R_ss[b]])
                        for s in range(4):
                            S.op("dve", lambda e, s=s, b=b: e.scalar_tensor_tensor(
                                out=xt[b][:, s, :], in0=xt[b][:, s, :], scalar=rstd[b][:, s:s + 1], in1=finbc[:],
                                op0=ALU.mult, op1=ALU.mult), reads=[R_xt[b], R_ss[b], R_const], writes=[R_xt[b]])
                        return lambda: S.dma("sp", lambda e: e.dma_start(
                            out=out_d[t * 512:(t + 1) * 512, :].rearrange("(s p) d -> p s d", p=128), in_=xt[b][:]),
                            reads=[R_xt[b]], writes=[R_out])

                sgc_ = [0]
                tmc_ = [0]
                load_x(0)
                issue_gu(NWG)
                issue_d(NWD)
                norm_router(0)
                pend = []
                for t in range(8):
                    def hook(t=t):
                        for st_ in pend:
                            st_()
                        pend.clear()
                        if t + 1 < 8:
                            load_x(t + 1)
                    experts(t, hook)
                    if t + 1 < 8:
                        norm_router(t + 1)
                    down(t)
                    pend.append(finish(t))
                for st_ in pend:
                    st_()
                S.barrier()
                S.emit()

        if nphase >= 2:
            moe_phase(0, final=(nphase == 2))

        if nphase >= 3:
            with ExitStack() as st:
                T, P = mkTP(st)

                win_sb = T("win_sb", [128, 8, 4096], BF16)
                bwoc = [T(f"bwoc{i}", [128, 4, 512], BF16) for i in range(3)]
                junk2 = T("junk2", [128, DM], BF16)
                ws_f = T("ws_f", [128, 8, 128], F32)
                ws_b = T("ws_b", [128, 8, 128], BF16)
                wsT = T("wsT", [128, 8, 128], BF16)
                bs_f = T("bs_f", [1, 1024], F32)
                bs_b = T("bs_b", [1, 1024], BF16)
                ones = T("ones", [1, 128], BF16)
                gain_f = T("gain_f", [128, 512], F32)
                gain = T("gain", [128, 2048], BF16)
                xt = [T(f"xt{i}", [128, 4, DM], F32) for i in range(2)]
                hT = T("hT", [128, 8, 512], BF16)
                ss = T("ss", [128, 4], F32)
                rstd = T("rstd", [128, 4], F32)
                uT = T("uT", [128, 16, 512], BF16)
                vf0 = T("vf0", [128, 2048], F32)
                vf = [vf0, vf0]
                xn = vf0[:].bitcast(BF16).rearrange("p (s d) -> p s d", d=DM)
                vn = T("vn", [128, 4, 2048], BF16)
                junk = vn[:, 3, :]
                st_ = T("st_", [128, 16], F32)
                tmp = [T(f"tmp{i}", [128, 512], F32) for i in range(2)]
                pT = [P(f"pT{i}", [128, 1024], BF16) for i in range(2)]
                pm = [P(f"pm{i}", [128, 512], F32) for i in range(6)]
                R_win, R_ws, R_gain = Reg("win"), Reg("ws"), Reg("gain")
                R_bwoc = [Reg(f"bwoc{i}") for i in range(3)]
                R_junk2 = Reg("junk2")
                R_xt = [Reg("xt0"), Reg("xt1")]
                R_hT, R_ss = Reg("hT"), Reg("ss")
                R_uT = [Reg(f"uT{c}") for c in range(16)]
                R_vf = [Reg("vf0")] * 2
                R_xn = R_vf[0]
                R_vn = [Reg(f"vn{s}") for s in range(4)]
                R_junk = R_vn[3]
                R_st = Reg("st")
                R_tmp = [Reg("tmp0"), Reg("tmp1")]
                R_pT = [Reg("pT0"), Reg("pT1")]
                R_pm = [Reg(f"pm{i}") for i in range(6)]

                for k in range(8):
                    S.dma("sp", lambda e, k=k: e.dma_start(out=win_sb[:, k, :], in_=win_b[k * 128:(k + 1) * 128, :]),
                          reads=[R_w["win_b"]], writes=[R_win])
                S.dma("sp", lambda e: e.dma_start(out=ws_f[:], in_=ws.rearrange("g p q -> p g q")), writes=[R_ws])
                S.dma("sp", lambda e: e.dma_start(out=bs_f[:], in_=bsr), writes=[R_ws])
                S.dma("sp", lambda e: e.dma_start(out=ones[:], in_=ones_d), writes=[R_ws])
                for q4 in range(4):
                    S.dma("sp", lambda e, q4=q4: e.dma_start(out=gain_f[:], in_=vgain[:, q4 * 512:(q4 + 1) * 512].partition_broadcast(128)),
                          writes=[R_gain])
                    S.op("dve", lambda e, q4=q4: e.tensor_copy(out=gain[:, q4 * 512:(q4 + 1) * 512], in_=gain_f[:]), reads=[R_gain], writes=[R_gain])
                S.op("dve", lambda e: e.tensor_copy(out=ws_b[:], in_=ws_f[:]), reads=[R_ws], writes=[R_ws])
                S.op("dve", lambda e: e.tensor_copy(out=bs_b[:], in_=bs_f[:]), reads=[R_ws], writes=[R_ws])
                for g in range(8):
                    S.op("pe", lambda e, g=g: e.transpose(pT[0][:, g * 128:(g + 1) * 128], ws_b[:, g, :], ident[:]),
                         reads=[R_ws, R_const], writes=[R_pT[0]] if g == 0 else [], sig=(g == 7))
                S.op("act", lambda e: e.copy(out=wsT[:], in_=pT[0][:].rearrange("p (g q) -> p g q", q=128)),
                     reads=[R_pT[0]], writes=[R_ws])
                a0, s0 = A_of(1, "m"), sh_of(1, "m")
                gi = gt_idx(1, "m")
                pmc_ = [0]
                tmc_ = [0]
                bw_next = [0]
                NBW = 3

                def issue_bw(upto):
                    while bw_next[0] < min(upto, 64):
                        i = bw_next[0]
                        bw_next[0] += 1
                        j, nbk, di = i % 4, (i // 4) % 2, i % NBW
                        S.dma("sp", lambda e: e.dma_start(
                            out=bwoc[di][:], in_=bwo_b[j * 512:(j + 1) * 512, nbk * 512:(nbk + 1) * 512].rearrange("(c p) n -> p c n", p=128)),
                            reads=[R_w["bwo_b"]], writes=[R_bwoc[di]])

                def g_load(t):
                    b = t % 2
                    S.dma("sp", lambda e: e.dma_start(
                        out=xt[b][:], in_=resid[t * 512:(t + 1) * 512, :].rearrange("(s p) d -> p s d", p=128)),
                        reads=[R_res[t]], writes=[R_xt[b]])

                def g_norm(t):
                    b = t % 2
                    norm_transpose(xt[b], R_xt[b], xn, R_xn, hT, R_hT, pT, R_pT, junk2, R_junk2, ss, rstd, R_ss, Av[:, 8:16], modv[:, 48:56])

                def g_v(t):
                    for s in range(4):
                        vb = s % 2
                        for nbk in range(4):
                            pb = pmc_[0] % 6
                            pmc_[0] += 1
                            for k in range(8):
                                S.op("pe", lambda e, s=s, k=k, nbk=nbk, pb=pb: e.matmul(
                                    pm[pb][:], lhsT=hT[:, k, s * 128:(s + 1) * 128],
                                    rhs=win_sb[:, k, 2048 + nbk * 512:2048 + (nbk + 1) * 512], start=(k == 0), stop=(k == 7)),
                                    reads=[R_hT, R_win], writes=[R_pm[pb]] if k == 0 else [], sig=(k == 7))
                            S.op("act", lambda e, nbk=nbk, pb=pb, vb=vb: e.activation(
                                out=vf[vb][:, nbk * 512:(nbk + 1) * 512], in_=pm[pb][:], func=AF.Gelu,
                                accum_out=st_[:, nbk:nbk + 1]), reads=[R_pm[pb]], writes=[R_vf[vb], R_st])
                        S.op("act", lambda e, vb=vb: e.activation(out=vn[:, s, :], in_=vf[vb][:], func=AF.Square, accum_out=st_[:, 4:5]),
                             reads=[R_vf[vb]], writes=[R_vn[s], R_st])
                        S.op("dve", lambda e: e.tensor_reduce(out=st_[:, 5:6], in_=st_[:, 0:4], axis=AX.X, op=ALU.add), reads=[R_st], writes=[R_st])
                        S.op("dve", lambda e: e.tensor_scalar(out=st_[:, 5:6], in0=st_[:, 5:6], scalar1=1.0 / 2048, scalar2=None, op0=ALU.mult),
                             reads=[R_st], writes=[R_st])
                        S.op("dve", lambda e: e.tensor_tensor(out=st_[:, 6:7], in0=st_[:, 5:6], in1=st_[:, 5:6], op=ALU.mult), reads=[R_st], writes=[R_st])
                        S.op("dve", lambda e: e.scalar_tensor_tensor(out=st_[:, 7:8], in0=st_[:, 4:5], scalar=1.0 / 2048, in1=st_[:, 6:7],
                                                                      op0=ALU.mult, op1=ALU.subtract), reads=[R_st], writes=[R_st])
                        S.op("act", lambda e: e.activation(out=st_[:, 8:9], in_=st_[:, 7:8], func=AF.Sqrt, bias=EPS), reads=[R_st], writes=[R_st])
                        S.op("dve", lambda e: e.reciprocal(out=st_[:, 8:9], in_=st_[:, 8:9]), reads=[R_st], writes=[R_st])
                        S.op("dve", lambda e, vb=vb: e.tensor_scalar(out=vf[vb][:], in0=vf[vb][:], scalar1=st_[:, 5:6], scalar2=st_[:, 8:9],
                                                                      op0=ALU.subtract, op1=ALU.mult), reads=[R_st, R_vf[vb]], writes=[R_vf[vb]])
                        S.op("pool", lambda e, vb=vb, s=s: e.tensor_tensor(out=vn[:, s, :], in0=vf[vb][:], in1=gain[:], op=ALU.mult),
                             reads=[R_vf[vb], R_gain], writes=[R_vn[s]])

                def g_u(t):
                    for mt in range(16):
                        pb = pmc_[0] % 6
                        pmc_[0] += 1
                        for k in range(8):
                            S.op("pe", lambda e, mt=mt, k=k, pb=pb: e.matmul(
                                pm[pb][:], lhsT=win_sb[:, k, mt * 128:(mt + 1) * 128], rhs=hT[:, k, :], start=(k == 0), stop=(k == 7)),
                                reads=[R_win, R_hT], writes=[R_pm[pb]] if k == 0 else [], sig=(k == 7))
                        S.op("act", lambda e, mt=mt, pb=pb: e.activation(out=uT[:, mt, :], in_=pm[pb][:], func=AF.Gelu),
                             reads=[R_pm[pb]], writes=[R_uT[mt]])

                def g_spatial(t):
                    for cc in range(16):
                        g = cc // 2
                        pb = pmc_[0] % 6
                        pmc_[0] += 1
                        for s in range(4):
                            S.op("pe", lambda e, cc=cc, s=s, g=g, pb=pb: e.matmul(
                                pm[pb][:, s * 128:(s + 1) * 128], lhsT=vn[:, s, cc * 128:(cc + 1) * 128], rhs=wsT[:, g, :],
                                start=True, stop=False), reads=[R_vn[s], R_ws], writes=[R_pm[pb]] if s == 0 else [], sig=False)
                            S.op("pe", lambda e, s=s, g=g, pb=pb: e.matmul(
                                pm[pb][:, s * 128:(s + 1) * 128], lhsT=ones[:], rhs=bs_b[:, g * 128:(g + 1) * 128],
                                start=False, stop=True), reads=[R_ws], writes=[], sig=(s == 3))
                        S.op("dve", lambda e, cc=cc, pb=pb: e.tensor_tensor(out=uT[:, cc, :], in0=uT[:, cc, :], in1=pm[pb][:], op=ALU.mult),
                             reads=[R_pm[pb], R_uT[cc]], writes=[R_uT[cc]])

                def g_out(t):
                    b = t % 2
                    for nbk in range(2):
                        for j in range(4):
                            i = t * 8 + nbk * 4 + j
                            di = i % NBW
                            issue_bw(i + 1)
                            for c4 in range(4):
                                cc = j * 4 + c4
                                for s in range(4):
                                    S.op("pe", lambda e: e.matmul(
                                        pm[s][:], lhsT=uT[:, cc, s * 128:(s + 1) * 128], rhs=bwoc[di][:, c4, :],
                                        start=(cc == 0), stop=(cc == 15)),
                                        reads=[R_uT[cc], R_bwoc[di]], writes=[R_pm[s]] if cc in (0, 15) else [],
                                        sig=(cc == 15 or (c4 == 3 and s == 3)))
                            issue_bw(i + NBW + 1)
                        for s in range(4):
                            ti = tmc_[0] % 2
                            tmc_[0] += 1
                            S.op("dve", lambda e: e.tensor_tensor(
                                out=tmp[ti][:], in0=pm[s][:], in1=gtbc[:, gi, nbk * 512:(nbk + 1) * 512], op=ALU.mult),
                                reads=[R_pm[s], R_gtbc], writes=[R_tmp[ti]])
                            S.op("pool", lambda e: e.tensor_tensor(
                                out=xt[b][:, s, nbk * 512:(nbk + 1) * 512], in0=xt[b][:, s, nbk * 512:(nbk + 1) * 512], in1=tmp[ti][:], op=ALU.add),
                                reads=[R_tmp[ti], R_xt[b]], writes=[R_xt[b]])
                    S.dma("sp", lambda e: e.dma_start(
                        out=resid[t * 512:(t + 1) * 512, :].rearrange("(s p) d -> p s d", p=128), in_=xt[b][:]),
                        reads=[R_xt[b]], writes=[R_res[t]])

                g_load(0)
                issue_bw(NBW)
                g_norm(0)
                for t in range(8):
                    if t + 1 < 8:
                        g_load(t + 1)
                    g_v(t)
                    g_u(t)
                    if t + 1 < 8:
                        g_norm(t + 1)
                    g_spatial(t)
                    g_out(t)
                S.barrier()
                S.emit()

        if nphase >= 4:
            moe_phase(1, final=True)

        S.barrier(final=True)
        S.emit()
    return nc


_CACHE = {}


def _consts():
    bf = ml_dtypes.bfloat16
    ident = np.eye(128, dtype=np.float32).astype(bf)
    kp = np.arange(128)[:, None]
    tq = np.arange(256)[None, :]
    band = (((tq - kp) >= 0) & ((tq - kp) <= 128)).astype(np.float32).astype(bf)
    ones = np.ones((1, 128), np.float32).astype(bf)
    return ident, band, ones


def _prep_inputs(inp):
    f32 = np.float32
    x = np.asarray(inp["x"], f32)
    c = np.asarray(inp["c"], f32)
    ident, band, ones = _consts()
    inv_freq = (np.float32(500000.0) ** (-(np.arange(0, 16, 2, dtype=np.float32)) / np.float32(16))).astype(f32)
    b_ada = np.asarray(inp["b_ada"], f32)
    b_ada_l = np.ascontiguousarray(b_ada.reshape(2, 48, 128).transpose(2, 0, 1).reshape(128, 96))
    nrm = np.concatenate([np.asarray(inp["norm_mix"], f32), np.asarray(inp["norm_ffn"], f32),
                          np.asarray(inp["final_norm"], f32)[None]], 0)
    nrm_l = np.ascontiguousarray(nrm.reshape(5, 8, 128).transpose(2, 0, 1).reshape(128, 40))
    wr = np.concatenate([np.asarray(inp["r_w_group"], f32),
                         np.asarray(inp["r_w_expert"], f32).transpose(0, 2, 1, 3).reshape(2, DM, 16)], -1)
    brr = np.concatenate([np.asarray(inp["r_b_group"], f32), np.asarray(inp["r_b_expert"], f32).reshape(2, 16)], -1).reshape(1, 40)
    shared = dict(
        w_ada=np.ascontiguousarray(inp["w_ada"], f32), b_ada_l=b_ada_l, b_ada_r=np.ascontiguousarray(b_ada),
        nrm=nrm_l, fin_row=np.asarray(inp["final_norm"], f32).reshape(1, DM),
        wqkv=np.ascontiguousarray(inp["a_w_qkv"][0], f32), wo=np.ascontiguousarray(inp["a_w_o"][0], f32),
        win=np.ascontiguousarray(inp["b_w_in"][0], f32), vgain=np.asarray(inp["b_v_gain"], f32).reshape(1, 2048),
        ws=np.ascontiguousarray(inp["b_w_s"][0], f32), bsr=np.asarray(inp["b_b_s"][0], f32).reshape(1, 1024),
        bwo=np.ascontiguousarray(inp["b_w_o"][0], f32), wr=np.ascontiguousarray(wr), brr=np.ascontiguousarray(brr),
        eg=np.ascontiguousarray(inp["e_w_gate"], f32), eu=np.ascontiguousarray(inp["e_w_up"], f32),
        ed=np.ascontiguousarray(inp["e_w_down"], f32), ident=ident, band=band, ones=ones,
        maskA=(np.arange(DM)[None, :] % 128 == np.arange(128)[:, None]).astype(f32),
        maskB=(np.arange(DM)[None, :] // 8 == np.arange(128)[:, None]).astype(f32),
        nrm2=np.ascontiguousarray(np.asarray(inp["norm_ffn"], f32).reshape(2, 128, 8).transpose(1, 0, 2).reshape(128, 16)),
    )
    maps = []
    for ci in range(NCORES):
        b, T0 = ci // 4, (ci % 4) * TOWN
        pos = T0 - HALO + np.arange(TEXT)
        valid = (pos >= 0) & (pos < SEQ)
        xe = np.zeros((TEXT, DM), f32)
        xe[valid] = x[b, pos[valid]]
        ang = pos.astype(f32)[:, None] * inv_freq[None, :]
        cs = np.concatenate([np.cos(ang), np.sin(ang)], -1).astype(f32)
        kval = np.ascontiguousarray(valid.astype(f32).reshape(48, 128).T)
        m = dict(shared)
        m.update(xext=xe, cvec=np.ascontiguousarray(c[b].reshape(8, 128).T), cs=cs, kval=kval)
        maps.append(m)
    return maps


def kernel(**inputs):
    if "nc" not in _CACHE:
        _CACHE["nc"] = build_program()
    nc = _CACHE["nc"]
    maps = _prep_inputs(inputs)
    res = run_bass_kernel_spmd(nc, maps, core_ids=list(range(NCORES)))
    out = np.empty((2, SEQ, DM), np.float32)
    for ci in range(NCORES):
        b, T0 = ci // 4, (ci % 4) * TOWN
        out[b, T0:T0 + TOWN] = res.results[ci]["out"]
    return out
```
